# Optimizing a Trainium2 kernel written in Bass

```python
import jax
import jax.numpy as jnp
from jax import lax

D_MODEL = 4096
BATCH = 1
SEQ = 16384
DEPTH = 4

GRID_W = 64
CTX_LEN = 256
ROPE_BASE = 10000.0
NORM_EPS = 1e-6
NEG_INF = -1e30

A_HEADS = 16
A_KV_HEADS = 4
A_HEAD_DIM = 128
A_WINDOW = 128
A_BLOCK = 128
B_CHANNELS = 2048
B_CONV_W = 31
C_HEADS = 32
C_Q_RANK = 1024
C_KV_RANK = 512
C_NOPE = 128
C_ROPE = 64
C_VDIM = 128
C_QBLOCK = 128
N_EXPERTS = 16
EXPERT_FF = 384
EC_CAPACITY = 2

A_Q = A_HEADS * A_HEAD_DIM
A_KV = A_KV_HEADS * A_HEAD_DIM
EVEN_IN = A_Q + 2 * A_KV + 2 * B_CHANNELS
EVEN_OUT = A_Q + B_CHANNELS
ODD_DOWN = C_Q_RANK + C_KV_RANK + C_ROPE
ODD_OUT = C_HEADS * C_VDIM

kernel_name = 'hybrid_swa_conformer_mla_ecmoe_diffusion_trunk'


def rmsnorm(x, g):
    xf = x.astype(jnp.float32)
    y = xf * lax.rsqrt(jnp.mean(xf * xf, axis=-1, keepdims=True) + NORM_EPS)
    return (y * g.astype(jnp.float32)).astype(x.dtype)


def layernorm(x, g, b):
    xf = x.astype(jnp.float32)
    mu = jnp.mean(xf, axis=-1, keepdims=True)
    var = jnp.mean(jnp.square(xf - mu), axis=-1, keepdims=True)
    y = (xf - mu) * lax.rsqrt(var + NORM_EPS)
    return (y * g.astype(jnp.float32) + b.astype(jnp.float32)).astype(x.dtype)


def modulate(h, shift, scale):
    return h * (1 + scale[:, None, :]) + shift[:, None, :]


def rope_pairs(x, cos, sin):
    m = x.shape[-1] // 2
    x1, x2 = x[..., :m], x[..., m:]
    return jnp.concatenate([x1 * cos - x2 * sin, x2 * cos + x1 * sin], axis=-1)


def axial_rope(x, rows, cols):
    half = x.shape[-1] // 2
    inv = ROPE_BASE ** (-jnp.arange(0, half, 2, dtype=jnp.float32) / half)
    ang_r = rows[:, None] * inv[None, :]
    ang_c = cols[:, None] * inv[None, :]
    cr = jnp.cos(ang_r)[None, :, None, :].astype(x.dtype)
    sr = jnp.sin(ang_r)[None, :, None, :].astype(x.dtype)
    cc = jnp.cos(ang_c)[None, :, None, :].astype(x.dtype)
    scn = jnp.sin(ang_c)[None, :, None, :].astype(x.dtype)
    return jnp.concatenate([rope_pairs(x[..., :half], cr, sr), rope_pairs(x[..., half:], cc, scn)], axis=-1)


def band_blocks(t, nb):
    b = t.shape[0]
    tb = t.reshape((b, nb, A_BLOCK) + t.shape[2:])
    tp = jnp.pad(tb, ((0, 0), (1, 1)) + ((0, 0),) * (tb.ndim - 2))
    return jnp.concatenate([tp[:, :-2], tp[:, 1:-1], tp[:, 2:]], axis=2)


def window_context_gqa(q, k, v, k_ctx, v_ctx, sink):
    b, n, h, dh = q.shape
    hkv = k.shape[2]
    g = h // hkv
    nb = n // A_BLOCK
    lc = k_ctx.shape[1]
    w3 = 3 * A_BLOCK
    scale = dh ** -0.5
    qb = q.reshape(b, nb, A_BLOCK, hkv, g, dh)
    kb = band_blocks(k, nb)
    vb = band_blocks(v, nb)
    s_loc = jnp.einsum('bnqhgd,bnkhd->bnhgqk', qb, kb).astype(jnp.float32) * scale
    blk = jnp.arange(nb)[:, None, None]
    qpos = blk * A_BLOCK + jnp.arange(A_BLOCK)[None, :, None]
    kpos = (blk - 1) * A_BLOCK + jnp.arange(w3)[None, None, :]
    valid = (jnp.abs(kpos - qpos) <= A_WINDOW) & (kpos >= 0) & (kpos < n)
    s_loc = jnp.where(valid[None, :, None, None], s_loc, NEG_INF)
    s_ctx = jnp.einsum('bnqhgd,bchd->bnhgqc', qb, k_ctx).astype(jnp.float32) * scale
    s_sink = jnp.broadcast_to(sink.astype(jnp.float32).reshape(1, 1, hkv, g, 1, 1), s_loc.shape[:-1] + (1,))
    p = jax.nn.softmax(jnp.concatenate([s_loc, s_ctx, s_sink], axis=-1), axis=-1)
    p_loc = p[..., :w3].astype(v.dtype)
    p_ctx = p[..., w3:w3 + lc].astype(v.dtype)
    o = jnp.einsum('bnhgqk,bnkhd->bnqhgd', p_loc, vb) + jnp.einsum('bnhgqc,bchd->bnqhgd', p_ctx, v_ctx)
    return o.reshape(b, n, h * dh)


def context_gqa(q, k, v, sink):
    b, l, h, dh = q.shape
    hkv = k.shape[2]
    g = h // hkv
    qg = q.reshape(b, l, hkv, g, dh)
    s = jnp.einsum('bqhgd,bkhd->bhgqk', qg, k).astype(jnp.float32) * dh ** -0.5
    s_sink = jnp.broadcast_to(sink.astype(jnp.float32).reshape(1, hkv, g, 1, 1), s.shape[:-1] + (1,))
    p = jax.nn.softmax(jnp.concatenate([s, s_sink], axis=-1), axis=-1)[..., :-1].astype(v.dtype)
    return jnp.einsum('bhgqk,bkhd->bqhgd', p, v).reshape(b, l, h * dh)


def conformer_conv(a, gate, dw, ln_g, ln_b):
    u = a * jax.nn.sigmoid(gate)
    ch = u.shape[-1]
    pad = (dw.shape[0] - 1) // 2
    z = lax.conv_general_dilated(u, dw[:, None, :].astype(u.dtype), window_strides=(1,), padding=[(pad, pad)],
                                 dimension_numbers=('NWC', 'WIO', 'NWC'), feature_group_count=ch)
    return jax.nn.silu(layernorm(z, ln_g, ln_b))


def even_mixer(hx, hy, w_in, sink, dw, ln_g, ln_b, w_out, rows, cols, need_ctx):
    b, n, _ = hx.shape
    lc = hy.shape[1]
    cuts = [A_Q, A_Q + A_KV, A_Q + 2 * A_KV, A_Q + 2 * A_KV + B_CHANNELS]
    qx, kx, vx, ax, gx = jnp.split(hx @ w_in, cuts, axis=-1)
    if need_ctx:
        qy, ky, vy, ay, gy = jnp.split(hy @ w_in, cuts, axis=-1)
    else:
        ky, vy = jnp.split(hy @ w_in[:, A_Q:A_Q + 2 * A_KV], 2, axis=-1)
    qx = axial_rope(qx.reshape(b, n, A_HEADS, A_HEAD_DIM), rows, cols)
    kx = axial_rope(kx.reshape(b, n, A_KV_HEADS, A_HEAD_DIM), rows, cols)
    vx = vx.reshape(b, n, A_KV_HEADS, A_HEAD_DIM)
    ky = ky.reshape(b, lc, A_KV_HEADS, A_HEAD_DIM)
    vy = vy.reshape(b, lc, A_KV_HEADS, A_HEAD_DIM)
    att_x = window_context_gqa(qx, kx, vx, ky, vy, sink)
    conv_x = conformer_conv(ax, gx, dw, ln_g, ln_b)
    ox = jnp.concatenate([att_x, conv_x], axis=-1) @ w_out
    oy = None
    if need_ctx:
        att_y = context_gqa(qy.reshape(b, lc, A_HEADS, A_HEAD_DIM), ky, vy, sink)
        conv_y = conformer_conv(ay, gy, dw, ln_g, ln_b)
        oy = jnp.concatenate([att_y, conv_y], axis=-1) @ w_out
    return ox, oy


def mla_queries(dq, q_norm_g, w_uq, rows, cols):
    b, l, _ = dq.shape
    q = (rmsnorm(dq, q_norm_g) @ w_uq).reshape(b, l, C_HEADS, C_NOPE + C_ROPE)
    qn, qr = q[..., :C_NOPE], q[..., C_NOPE:]
    if rows is not None:
        qr = axial_rope(qr, rows, cols)
    return qn, qr


def mla_keys(dkv, kv_norm_g, w_ukv, rows, cols):
    b, l, _ = dkv.shape
    ckv = rmsnorm(dkv[..., :C_KV_RANK], kv_norm_g)
    kr = dkv[..., C_KV_RANK:][:, :, None, :]
    if rows is not None:
        kr = axial_rope(kr, rows, cols)
    kv = (ckv @ w_ukv).reshape(b, l, C_HEADS, C_NOPE + C_VDIM)
    return kv[..., :C_NOPE], kr[:, :, 0, :], kv[..., C_NOPE:]


def mla_attend(qn, qr, kn, kr, v):
    b, l, h, _ = qn.shape
    nb = l // C_QBLOCK
    scale = (C_NOPE + C_ROPE) ** -0.5

    def to_blocks(t):
        return jnp.moveaxis(t.reshape((b, nb, C_QBLOCK) + t.shape[2:]), 1, 0)

    def attend(qb):
        qn_b, qr_b = qb
        s = (jnp.einsum('bqhd,bkhd->bhqk', qn_b, kn) + jnp.einsum('bqhr,bkr->bhqk', qr_b, kr)).astype(jnp.float32) * scale
        p = jax.nn.softmax(s, axis=-1).astype(v.dtype)
        return jnp.einsum('bhqk,bkhd->bqhd', p, v)

    o = lax.map(attend, (to_blocks(qn), to_blocks(qr)))
    return jnp.moveaxis(o, 0, 1).reshape(b, l, h * C_VDIM)


def mla_mixer(hx, hy, w_dn, q_norm_g, kv_norm_g, w_uq, w_ukv, w_o, rows, cols, need_ctx):
    dx = hx @ w_dn
    qxn, qxr = mla_queries(dx[..., :C_Q_RANK], q_norm_g, w_uq, rows, cols)
    kxn, kxr, vx = mla_keys(dx[..., C_Q_RANK:], kv_norm_g, w_ukv, rows, cols)
    dy = hy @ (w_dn if need_ctx else w_dn[:, C_Q_RANK:])
    kyn, kyr, vy = mla_keys(dy[..., -(C_KV_RANK + C_ROPE):], kv_norm_g, w_ukv, None, None)
    kn = jnp.concatenate([kxn, kyn], axis=1)
    kr = jnp.concatenate([kxr, kyr], axis=1)
    v = jnp.concatenate([vx, vy], axis=1)
    ox = mla_attend(qxn, qxr, kn, kr, v) @ w_o
    oy = None
    if need_ctx:
        qyn, qyr = mla_queries(dy[..., :C_Q_RANK], q_norm_g, w_uq, None, None)
        oy = mla_attend(qyn, qyr, kyn, kyr, vy) @ w_o
    return ox, oy


def ec_moe(h, w_router, w_gate, w_up, w_down):
    b, l, d = h.shape
    cap = EC_CAPACITY * l // N_EXPERTS
    aff = jax.nn.softmax(jnp.einsum('bld,de->ble', h, w_router).astype(jnp.float32), axis=-1)
    gate, idx = lax.top_k(jnp.swapaxes(aff, 1, 2), cap)
    xs = jax.vmap(lambda hb, ib: hb[ib])(h, idx)
    a = jnp.einsum('becd,edf->becf', xs, w_gate)
    u = jnp.einsum('becd,edf->becf', xs, w_up)
    y = jnp.einsum('becf,efd->becd', jax.nn.silu(a) * u, w_down) * gate[..., None].astype(h.dtype)

    def combine(ib, yb):
        return jnp.zeros((l, d), yb.dtype).at[ib.reshape(-1)].add(yb.reshape(-1, d))

    return jax.vmap(combine)(idx, y)


def setup_inputs(seed: int = 0) -> dict:
    key = jax.random.key(seed)
    ks = jax.random.split(key, 26)
    ne = (DEPTH + 1) // 2
    no = DEPTH // 2
    d = D_MODEL

    def nrm(k, shape, std):
        return jax.random.normal(k, shape, jnp.float32) * std

    def gain(k, shape):
        return 1.0 + 0.02 * jax.random.normal(k, shape, jnp.float32)

    return {
        'x': nrm(ks[0], (BATCH, SEQ, d), 1.0),
        'c': nrm(ks[1], (BATCH, d), 1.0),
        'ctx': nrm(ks[2], (BATCH, CTX_LEN, d), 1.0),
        'c_ctx': nrm(ks[3], (d,), 1.0),
        'ada_w': nrm(ks[4], (DEPTH, d, 6 * d), 0.5 * d ** -0.5),
        'ada_b': nrm(ks[5], (DEPTH, 6 * d), 0.02),
        'norm1_g': gain(ks[6], (DEPTH, d)),
        'norm2_g': gain(ks[7], (DEPTH, d)),
        'ev_w_in': nrm(ks[8], (ne, d, EVEN_IN), d ** -0.5),
        'ev_sink': nrm(ks[9], (ne, A_HEADS), 0.5),
        'ev_dw': nrm(ks[10], (ne, B_CONV_W, B_CHANNELS), B_CONV_W ** -0.5),
        'ev_ln_g': gain(ks[11], (ne, B_CHANNELS)),
        'ev_ln_b': nrm(ks[12], (ne, B_CHANNELS), 0.02),
        'ev_w_out': nrm(ks[13], (ne, EVEN_OUT, d), EVEN_OUT ** -0.5),
        'od_w_dn': nrm(ks[14], (no, d, ODD_DOWN), d ** -0.5),
        'od_q_norm_g': gain(ks[15], (no, C_Q_RANK)),
        'od_kv_norm_g': gain(ks[16], (no, C_KV_RANK)),
        'od_w_uq': nrm(ks[17], (no, C_Q_RANK, C_HEADS * (C_NOPE + C_ROPE)), C_Q_RANK ** -0.5),
        'od_w_ukv': nrm(ks[18], (no, C_KV_RANK, C_HEADS * (C_NOPE + C_VDIM)), C_KV_RANK ** -0.5),
        'od_w_o': nrm(ks[19], (no, ODD_OUT, d), ODD_OUT ** -0.5),
        'moe_router': nrm(ks[20], (DEPTH, d, N_EXPERTS), d ** -0.5),
        'moe_w_gate': nrm(ks[21], (DEPTH, N_EXPERTS, d, EXPERT_FF), d ** -0.5),
        'moe_w_up': nrm(ks[22], (DEPTH, N_EXPERTS, d, EXPERT_FF), d ** -0.5),
        'moe_w_down': nrm(ks[23], (DEPTH, N_EXPERTS, EXPERT_FF, d), EXPERT_FF ** -0.5),
        'final_g': gain(ks[24], (d,)),
    }


def reference(x, c, ctx, c_ctx, ada_w, ada_b, norm1_g, norm2_g, ev_w_in, ev_sink, ev_dw, ev_ln_g, ev_ln_b,
              ev_w_out, od_w_dn, od_q_norm_g, od_kv_norm_g, od_w_uq, od_w_ukv, od_w_o, moe_router, moe_w_gate,
              moe_w_up, moe_w_down, final_g):
    b, n, d = x.shape
    ROWS = n // GRID_W
    rows = jnp.repeat(jnp.arange(ROWS, dtype=jnp.float32), GRID_W)
    cols = (jnp.arange(ROWS * GRID_W) % GRID_W).astype(jnp.float32)
    s_lat = jax.nn.silu(c)
    s_ctx = jax.nn.silu(c_ctx)[None, :]
    y = ctx
    for l in range(DEPTH):
        need_ctx = l < DEPTH - 1
        mx = jnp.split(s_lat @ ada_w[l] + ada_b[l], 6, axis=-1)
        my = jnp.split(s_ctx @ ada_w[l] + ada_b[l], 6, axis=-1)
        hx = modulate(rmsnorm(x, norm1_g[l]), mx[0], mx[1])
        hy = modulate(rmsnorm(y, norm1_g[l]), my[0], my[1])
        i = l // 2
        if l % 2 == 0:
            ox, oy = even_mixer(hx, hy, ev_w_in[i], ev_sink[i], ev_dw[i], ev_ln_g[i], ev_ln_b[i], ev_w_out[i],
                                rows, cols, need_ctx)
        else:
            ox, oy = mla_mixer(hx, hy, od_w_dn[i], od_q_norm_g[i], od_kv_norm_g[i], od_w_uq[i], od_w_ukv[i],
                               od_w_o[i], rows, cols, need_ctx)
        x = x + mx[2][:, None, :] * ox
        hx = modulate(rmsnorm(x, norm2_g[l]), mx[3], mx[4])
        x = x + mx[5][:, None, :] * ec_moe(hx, moe_router[l], moe_w_gate[l], moe_w_up[l], moe_w_down[l])
        if need_ctx:
            y = y + my[2][:, None, :] * oy
            hy = modulate(rmsnorm(y, norm2_g[l]), my[3], my[4])
            y = y + my[5][:, None, :] * ec_moe(hy, moe_router[l], moe_w_gate[l], moe_w_up[l], moe_w_down[l])
    return rmsnorm(x, final_g)
```

```python
import contextlib
import numpy as np
import concourse.bass as bass
import concourse.mybir as mybir
from concourse.bass_utils import run_bass_kernel_spmd

F32 = mybir.dt.float32
BF16 = mybir.dt.bfloat16
AF = mybir.ActivationFunctionType
ALU = mybir.AluOpType
AX = mybir.AxisListType

NCORE = 8
CT = 256
DEPTH = 4
EPS = 1e-6
A_HEADS, A_KV, A_DH = 16, 4, 128
B_CH, B_W = 2048, 31
C_HEADS, C_QR, C_KVR, C_NOPE, C_ROPE, C_V = 32, 1024, 512, 128, 64, 128
NE, EFF = 16, 384
EVEN_IN = 7168
ODD_DOWN = 1600


class Buf:
    __slots__ = ("name", "w", "r", "dsem", "multi", "wm")

    def __init__(self, name="", multi=False):
        self.name = name
        self.w = None
        self.r = {}
        self.dsem = None
        self.multi = multi
        self.wm = {}


class T:
    def __init__(self, t, name, multi=False, rows=None):
        self.t = t
        self.b = Buf(name, multi)
        self.rows = rows

    def __getitem__(self, k):
        return self.t[k]

    def ap(self):
        if self.rows is not None:
            return self.t.ap()[0:self.rows]
        return self.t.ap()


class DSem:
    def __init__(self, sem):
        self.sem = sem
        self.cnt = 0


def _b(x):
    return x.b if isinstance(x, T) else x


class KB:
    def __init__(self, nc):
        self.nc = nc
        self.eng = {"pe": nc.tensor, "act": nc.scalar, "dve": nc.vector,
                    "pool": nc.gpsimd, "sp": nc.sync}
        self.esem, self.ecnt, self.known = {}, {}, {}
        for e in ("pe", "act", "dve", "pool"):
            self.esem[e] = nc.alloc_semaphore("es_" + e)
            self.ecnt[e] = 0
        for e in self.eng:
            self.known[e] = {}
        self.uid = 0
        self.free_ds = {}
        self.all_ds = []
        self.live = []
        self.multis = []
        self.ccsem = DSem(nc.alloc_semaphore("ccsem"))
        self.semname = {id(self.esem[e]): 'E_' + e for e in self.esem}
        self.semname[id(self.ccsem.sem)] = 'cc'

    def T_sb(self, st, name, shape, dt):
        self.uid += 1
        t = st.enter_context(self.nc.sbuf_tensor(f"{name}_{self.uid}", list(shape), dt))
        return T(t, name)

    def T_ps(self, name, shape, dt=F32):
        self.uid += 1
        t = self.nc.alloc_psum_tensor(f"{name}_{self.uid}", list(shape), dt)
        return T(t, name)

    def T_dram(self, name, shape, dt, kind="Internal", multi=True):
        shp = [shape[0] + 1] + list(shape[1:])
        t = self.nc.dram_tensor(name, shp, dt, kind=kind)
        r = T(t, name, multi=multi, rows=shape[0])
        if multi:
            self.multis.append(r.b)
        return r

    def _get_ds(self, b, q="sp"):
        qk = "pool" if q == "pool" else "hw"
        if b.dsem is None:
            fl = self.free_ds.setdefault(qk, [])
            if fl:
                b.dsem = fl.pop()
            else:
                self.uid += 1
                b.dsem = DSem(self.nc.alloc_semaphore(f"ds_{self.uid}"))
                b.dsem.qk = qk
                self.all_ds.append(b.dsem)
                self.semname[id(b.dsem.sem)] = f'ds{len(self.all_ds)}{qk}'
            self.live.append(b)
        assert b.dsem.qk == qk, f"buffer {b.name} written by both hw and sw dma queues"
        return b.dsem

    def _wait(self, e, tok):
        if tok is None:
            return
        sem, val = tok
        if e == "pe" and sem is self.esem["pe"]:
            return
        k = self.known[e]
        key = id(sem)
        if k.get(key, 0) >= val:
            return
        self.eng[e].wait_ge(sem, val)
        k[key] = val
        if getattr(self, 'trace', None) is not None:
            self.trace.append(f'  {e} WAIT {self.semname.get(key, key)} >= {val}')

    def _deps(self, e, reads, writes):
        for b in reads:
            self._wait(e, b.w)
            for tok in list(b.wm.values()):
                self._wait(e, tok)
        for b in writes:
            if not b.multi:
                self._wait(e, b.w)
            for tok in list(b.r.values()):
                self._wait(e, tok)

    def _record(self, tok, reads, writes):
        sem, val = tok
        for b in reads:
            old = b.r.get(id(sem))
            if old is None or old[1] < val:
                b.r[id(sem)] = tok
        for b in writes:
            if b.multi:
                b.wm[id(sem)] = tok
            else:
                b.w = tok
                b.r = {}

    def op(self, e, fn, reads=(), writes=(), sig=True):
        reads = [_b(x) for x in reads]
        writes = [_b(x) for x in writes]
        self._deps(e, reads, writes)
        ins = fn(self.eng[e])
        if getattr(self, 'trace', None) is not None:
            self.trace.append(f'{e} OP reads={[b.name for b in reads]} writes={[b.name for b in writes]} sig={sig} cnt={self.ecnt[e]}')
        if sig:
            self.ecnt[e] += 1
            ins.then_inc(self.esem[e], 1)
            tok = (self.esem[e], self.ecnt[e])
        else:
            tok = (self.esem[e], self.ecnt[e] + 1)
        self._record(tok, reads, writes)
        return tok

    def dma(self, q, out, in_, reads=(), writes=(), **kw):
        reads = [_b(x) for x in reads]
        writes = [_b(x) for x in writes]
        import os
        if q == "sp" and in_.dtype == BF16 and "DRam" in type(in_.tensor).__name__ and not int(os.environ.get("NOREROUTE", "0")):
            q = "pool"
        owner = reads[0] if (writes[0].multi and reads and not reads[0].multi) else writes[0]
        ds = self._get_ds(owner, q)
        self._deps(q, reads, writes)
        ins = self.eng[q].dma_start(out=out, in_=in_, **kw)
        if getattr(self, 'trace', None) is not None:
            self.trace.append(f'{q} DMA reads={[b.name for b in reads]} writes={[b.name for b in writes]} sem={self.semname.get(id(ds.sem))} -> {ds.cnt + 16}')
        ds.cnt += 16
        ins.then_inc(ds.sem, 16)
        tok = (ds.sem, ds.cnt)
        self._record(tok, reads, writes)
        return tok

    def allgather(self, src, dst):
        self._deps("pool", [src.b], [dst.b])
        ins = self.nc.gpsimd.collective_compute(
            "AllGather", ALU.bypass, replica_groups=[list(range(NCORE))],
            ins=[src.ap().opt()], outs=[dst.ap().opt()])
        self.ccsem.cnt += 1
        ins.then_inc(self.ccsem.sem, 1)
        tok = (self.ccsem.sem, self.ccsem.cnt)
        self._record(tok, [src.b], [dst.b])
        return tok

    def barrier(self):
        toks = [(self.esem[e], self.ecnt[e]) for e in self.esem if self.ecnt[e] > 0]
        toks += [(d.sem, d.cnt) for d in self.all_ds if d.cnt > 0]
        if self.ccsem.cnt:
            toks.append((self.ccsem.sem, self.ccsem.cnt))
        for e in self.eng:
            for tok in toks:
                self._wait(e, tok)
        for b in self.live:
            self.free_ds.setdefault(b.dsem.qk, []).append(b.dsem)
            b.dsem = None
            b.w = None
            b.r = {}
            b.wm = {}
        self.live = []
        for b in self.multis:
            b.wm = {}
            b.r = {}

    def mm(self, out, lhsT, rhs, start, stop, reads, writes, sig=None):
        return self.op("pe", lambda e: e.matmul(out, lhsT=lhsT, rhs=rhs, start=start, stop=stop),
                       reads, writes, sig=(True if sig is None else sig))

    def tr(self, out, in_, ident, reads, writes, sig=True):
        return self.op("pe", lambda e: e.transpose(out=out, in_=in_, identity=ident), reads, writes, sig=sig)

    def act(self, out, in_, func, reads, writes, **kw):
        return self.op("act", lambda e: e.activation(out=out, in_=in_, func=func, **kw), reads, writes)

    def tt(self, e, out, a, b, op, reads, writes):
        return self.op(e, lambda en: en.tensor_tensor(out=out, in0=a, in1=b, op=op), reads, writes)

    def ts(self, e, out, a, s1, s2, op0, op1, reads, writes, **kw):
        if s2 is None and not kw:
            return self.op(e, lambda en: en.tensor_scalar(out=out, in0=a, scalar1=s1, scalar2=None, op0=op0),
                           reads, writes)
        return self.op(e, lambda en: en.tensor_scalar(out=out, in0=a, scalar1=s1, scalar2=s2, op0=op0, op1=op1, **kw),
                       reads, writes)

    def stt(self, out, a, s, b, op0, op1, reads, writes):
        return self.op("dve", lambda en: en.scalar_tensor_tensor(out=out, in0=a, scalar=s, in1=b, op0=op0, op1=op1),
                       reads, writes)

    def cp(self, e, out, in_, reads, writes):
        if e == "act":
            return self.op("act", lambda en: en.copy(out=out, in_=in_), reads, writes)
        return self.op(e, lambda en: en.tensor_copy(out=out, in_=in_), reads, writes)

    def memset(self, e, ap, val, writes):
        return self.op(e, lambda en: en.memset(ap, val), [], writes)


class MK:
    def __init__(self, D, TL, debug=False):
        self.D, self.TL = D, TL
        self.KC = D // 128
        self.NT = TL // 128
        self.TS = TL + 512
        self.LAT = TL + 256
        self.SEQ = TL * NCORE
        self.DS = D // NCORE
        self.debug = debug
        self.nc = bass.Bass("TRN2", target_bir_lowering=False)
        self.kb = KB(self.nc)
        self.inputs = {}
        self.wq = 0

    def inp(self, name, shape, dt=F32, pad=False):
        shp = [shape[0] + 1] + list(shape[1:]) if pad else list(shape)
        t = self.nc.dram_tensor(name, shp, dt, kind="ExternalInput")
        self.inputs[name] = T(t, name, rows=shape[0] if pad else None)
        return self.inputs[name]

    def declare(self):
        D, TL, KC, TS = self.D, self.TL, self.KC, self.TS
        kb = self.kb
        i = self.inp
        self.x_own = i("x_own", [TL, D], pad=True)
        self.ctx_in = i("ctx", [CT, D], pad=True)
        self.cT = i("cT", [128, KC, 2])
        self.ada_w = i("ada_w_s", [DEPTH, D, 6 * self.DS])
        self.ada_b = i("ada_b_s", [DEPTH, 1, 6 * self.DS])
        self.norm_g = i("norm_g", [2 * DEPTH + 1, D])
        self.router = i("router", [DEPTH, D, NE])
        self.sink = i("sink", [2, A_HEADS])
        self.dwT = i("dwT", [2, 128, 16, B_W])
        self.lngT = i("lngT", [2, 128, 16])
        self.lnbT = i("lnbT", [2, 128, 16])
        self.qngT = i("qngT", [2, 128, 8])
        self.kvngT = i("kvngT", [2, 128, 4])
        self.c_ident = i("ident", [128, 128])
        self.c_rotE = i("rotE", [128, 128])
        self.c_rotM = i("rotM", [64, 64])
        self.c_m16 = i("m16", [128, 128])
        self.c_sel = i("sel16", [16, 16 * 128])
        self.c_triA = i("triA", [128, 512])
        self.c_triB = i("triB", [128, 512])
        self.c_cosE = i("cosE", [128, self.LAT])
        self.c_sinE = i("sinE", [128, self.LAT])
        self.c_cosM = i("cosM", [64, TL])
        self.c_sinM = i("sinM", [64, TL])
        self.c_tmask = i("tmask", [1, 256])
        self.c_kmask = i("kmask", [128, 2])
        self.c_ohp = i("ohp", [128, 8])
        self.c_ohn = i("ohn", [128, 8])
        self.wspec = {}
        def w(name, rows, cols, layers):
            for l in range(layers):
                loc32 = i(f"{name}{l}", [rows // NCORE, cols])
                self.wspec[(name, l)] = dict(
                    src=loc32, rows=rows, cols=cols,
                    loc=kb.T_dram(f"{name}{l}_lb", [rows // NCORE, cols], BF16, multi=False),
                    gat=kb.T_dram(f"{name}{l}_g", [rows, cols], BF16, multi=False))
        w("ev_w_in", D, EVEN_IN, 2)
        w("ev_w_out", 4096, D, 2)
        w("od_w_dn", D, ODD_DOWN, 2)
        w("od_w_uq", C_QR, C_HEADS * 192, 2)
        w("od_w_ukv", C_KVR, C_HEADS * 256, 2)
        w("od_w_o", 4096, D, 2)
        w("moe_g", NE * D, EFF, DEPTH)
        w("moe_u", NE * D, EFF, DEPTH)
        w("moe_d", NE * EFF, D, DEPTH)
        self.out = T(self.nc.dram_tensor("out", [TL, D], F32, kind="ExternalOutput"), "out", multi=True)
        kb.multis.append(self.out.b)
        d = kb.T_dram
        self.xres = [d("xresA", [TL, D], F32), d("xresB", [TL, D], F32)]
        self.yres = [d("yresA", [CT, D], F32), d("yresB", [CT, D], F32)]
        self.mods_loc = d("mods_loc", [2, DEPTH, 6, self.DS], F32, multi=False)
        self.mods_all = d("mods_all", [NCORE, 2, DEPTH, 6, self.DS], F32, multi=False)
        self.edge_loc = d("edge_loc", [256, D], F32, multi=False)
        self.edge_all = d("edge_all", [NCORE * 256, D], F32, multi=False)
        self.xhalo = d("xhalo", [256, D], F32)
        self.hxT = d("hxT", [KC, 128, TS], BF16)
        self.qT = d("qT", [A_HEADS, 128, TS], BF16)
        self.kT = d("kT", [A_KV, 128, TS], BF16)
        self.vE = d("vE", [TS, A_KV * 128], BF16)
        self.uT = d("uT", [16, 128, TS], F32)
        self.mixT = d("mixT", [32, 128, TS], BF16)
        self.otok = d("otok", [TS, 4096], BF16)
        self.aff_loc = d("aff_loc", [NE, TL], F32, multi=False)
        self.aff_all = d("aff_all", [NCORE * NE, TL], F32, multi=False)
        self.GT = d("GT", [NE, TS], F32)
        self.HT = d("HT", [48, 128, TS], BF16)
        self.dqT = d("dqT", [8, 128, TS], BF16)
        self.ckvT = d("ckvT", [4, 128, TS], BF16)
        self.rstdq = d("rstdq", [1, TS], F32)
        self.rstdkv = d("rstdkv", [1, TS], F32)
        self.qnT = d("qnT", [C_HEADS, 128, TS], BF16)
        self.qrT = d("qrT", [C_HEADS, 64, TS], BF16)
        self.kn_loc = d("kn_loc", [C_HEADS, 128, TL], BF16)
        self.kn_all = d("kn_all", [NCORE, C_HEADS, 128, TL], BF16, multi=False)
        self.kn_ctx = d("kn_ctx", [C_HEADS, 128, CT], BF16)
        self.v_loc = d("v_loc", [TL, 4096], BF16)
        self.v_all = d("v_all", [NCORE * TL, 4096], BF16, multi=False)
        self.v_ctx = d("v_ctx", [CT, 4096], BF16)
        self.kr_loc = d("kr_loc", [64, TL], BF16)
        self.kr_all = d("kr_all", [NCORE, 64, TL], BF16, multi=False)
        self.kr_ctx = d("kr_ctx", [64, CT], BF16)
        self.dbg = {}
        self.pm = [kb.T_ps(f"pm{j}", [128, 512]) for j in range(4)]
        self.po = [kb.T_ps(f"po{j}", [128, 512]) for j in range(2)]
        self.ptr = [kb.T_ps(f"ptr{j}", [128, 1024], BF16) for j in range(2)]
        self.pmi = 0
        self.poi = 0
        self.ptri = 0

    def next_pm(self):
        self.pmi = (self.pmi + 1) % 4
        return self.pm[self.pmi]

    def next_po(self):
        self.poi = (self.poi + 1) % 2
        return self.po[self.poi]

    def next_ptr(self):
        self.ptri = (self.ptri + 1) % 2
        return self.ptr[self.ptri]

    def own_groups(self):
        return [(s0, min(512, self.TL - s0)) for s0 in range(0, self.TL, 512)]

    def halo_group(self):
        return (self.TL, 256)

    def ctx_group(self):
        return (self.TL + 256, 256)

    def load_consts(self, st):
        kb = self.kb
        def ld(name, src, shape, dt=F32):
            t = kb.T_sb(st, name, shape, dt)
            kb.dma("sp", t[tuple(slice(None) for _ in shape)], src.ap(), reads=[src], writes=[t])
            return t
        self.ident = ld("ident", self.c_ident, [128, 128])
        self.identb = kb.T_sb(st, "identb", [128, 128], BF16)
        kb.cp("dve", self.identb[:, :], self.ident[:, :], [self.ident], [self.identb])
        self.rotE = ld("rotE", self.c_rotE, [128, 128])
        self.rotM = ld("rotM", self.c_rotM, [64, 64])
        self.m16 = ld("m16", self.c_m16, [128, 128])
        self.sel = ld("sel", self.c_sel, [16, 16 * 128])
        self.onesf = kb.T_sb(st, "onesf", [128, 128], F32)
        kb.memset("dve", self.onesf[:, :], 1.0, [self.onesf])
        self.onesb = kb.T_sb(st, "onesb", [128, 128], BF16)
        kb.memset("dve", self.onesb[:, :], 1.0, [self.onesb])

    def prep_weight(self, key):
        sp = self.wspec[key]
        if sp.get("done"):
            return
        sp["done"] = True
        kb = self.kb
        rows, cols = sp["rows"] // NCORE, sp["cols"]
        f = cols
        while f > 2048 or cols % f:
            f -= 1
            while cols % f:
                f -= 1
        n = rows * cols // f
        src = sp["src"].ap().rearrange("r (a f) -> (r a) f", f=f)
        dst = sp["loc"].ap().rearrange("r (a f) -> (r a) f", f=f)
        step = 4096
        for i0 in range(0, n, step):
            i1 = min(n, i0 + step)
            kb.dma("pool", dst[i0:i1, :], src[i0:i1, :], reads=[sp["src"]], writes=[sp["loc"]])
        kb.allgather(sp["loc"], sp["gat"])

    def prep_layer_weights(self, l):
        i = l // 2
        if l % 2 == 0:
            names = [("ev_w_in", i), ("ev_w_out", i)]
        else:
            names = [("od_w_dn", i), ("od_w_uq", i), ("od_w_ukv", i), ("od_w_o", i)]
        names += [("moe_g", l), ("moe_u", l), ("moe_d", l)]
        for k in names:
            self.prep_weight(k)

    def W(self, name, l):
        return self.wspec[(name, l)]["gat"]

    def phase_ada(self):
        kb, KC, DS = self.kb, self.KC, self.DS
        NB = 6 * DS
        bw = min(NB, 256)
        with contextlib.ExitStack() as st:
            cs = kb.T_sb(st, "cs", [128, KC, 2], F32)
            ss = kb.T_sb(st, "ss", [128, KC, 2], F32)
            kb.dma("sp", cs[:, :, :], self.cT.ap(), reads=[self.cT], writes=[cs])
            kb.act(ss[:, :, :], cs[:, :, :], AF.Silu, [cs], [ss])
            wts = [kb.T_sb(st, f"adaw{j}", [128, KC, bw], F32) for j in range(2)]
            bts = [kb.T_sb(st, f"adab{j}", [1, bw], F32) for j in range(2)]
            res = kb.T_sb(st, "adares", [2, DEPTH, NB], F32)
            it = 0
            for l in range(DEPTH):
                for n0 in range(0, NB, bw):
                    cw = min(bw, NB - n0)
                    wt, bt = wts[it % 2], bts[it % 2]
                    it += 1
                    kb.dma("sp", wt[:, :, 0:cw], self.ada_w.ap()[l, :, n0:n0 + cw].rearrange("(c p) n -> p c n", p=128),
                           reads=[self.ada_w], writes=[wt])
                    kb.dma("sp", bt[:, 0:cw], self.ada_b.ap()[l, :, n0:n0 + cw], reads=[self.ada_b], writes=[bt])
                    ps = self.next_pm()
                    for kc in range(KC):
                        kb.mm(ps[0:2, 0:cw], ss[:, kc, :], wt[:, kc, 0:cw], kc == 0, False, [ss, wt], [ps], sig=False)
                    kb.mm(ps[0:2, 0:cw], self.onesf[0:1, 0:2], bt[:, 0:cw], False, True, [self.onesf, bt], [ps])
                    kb.cp("act", res[:, l, n0:n0 + cw], ps[0:2, 0:cw], [ps], [res])
            kb.dma("pool", self.mods_loc.ap().rearrange("w l j d -> w l (j d)"), res[:, :, :], reads=[res], writes=[self.mods_loc])
            kb.allgather(self.mods_loc, self.mods_all)
            kb.barrier()

    def mod_vec(self, w, l, j):
        return self.mods_all.ap()[:, w, l, j, :]

    def load_bc(self, q, dst, vec_ap, reads):
        kb = self.kb
        if len(vec_ap.shape) == 2:
            kb.dma(q, dst[:, :].rearrange("p (r d) -> p r d", r=NCORE), vec_ap.partition_broadcast(128),
                   reads=reads, writes=[dst])
        else:
            kb.dma(q, dst[:, :], vec_ap.partition_broadcast(128), reads=reads, writes=[dst])

    def phase_norm(self, srcs, l, gi, jshift, jscale, router=None):
        kb, D, KC = self.kb, self.D, self.KC
        with contextlib.ExitStack() as st:
            A = kb.T_sb(st, "nA", [128, D], F32)
            Bt = kb.T_sb(st, "nB", [128, D], F32)
            xts = [kb.T_sb(st, f"nx{j}", [128, D], F32) for j in range(2)]
            hbs = [kb.T_sb(st, f"nh{j}", [128, D], BF16) for j in range(2)]
            stg = [kb.T_sb(st, f"nst{j}", [128, KC, 256], BF16) for j in range(2)]
            sq = kb.T_sb(st, "nsq", [128, D], BF16)
            stat = [kb.T_sb(st, f"nstat{j}", [128, 4], F32) for j in range(2)]
            if router is not None:
                h32T = kb.T_sb(st, "h32T", [128, KC, 128], F32)
                wr = kb.T_sb(st, "wr", [128, KC, NE], F32)
                kb.dma("sp", wr[:, :, :], self.router.ap()[l].rearrange("(c p) e -> p c e", p=128),
                       reads=[self.router], writes=[wr])
                rs = [kb.T_sb(st, f"rs{j}", [128, 64], F32) for j in range(2)]
            it = 0
            sti = 0
            for which in (0, 1):
                mine = [s for s in srcs if s[0] == which]
                if not mine:
                    continue
                tmp = xts[0]
                self.load_bc("sp", A, self.norm_g.ap()[gi, :], [self.norm_g])
                self.load_bc("sp", tmp, self.mod_vec(which, l, jscale), [self.mods_all])
                self.load_bc("sp", Bt, self.mod_vec(which, l, jshift), [self.mods_all])
                kb.stt(A[:, :], tmp[:, :], 1.0, A[:, :], ALU.add, ALU.mult, [tmp, A], [A])
                for (_, src, row0, s0, ntiles) in mine:
                    for t in range(ntiles):
                        xt, hb, sta = xts[it % 2], hbs[it % 2], stat[it % 2]
                        it += 1
                        r0 = row0 + t * 128
                        kb.dma("sp", xt[:, :], src.ap()[r0:r0 + 128, :], reads=[src], writes=[xt])
                        kb.act(sq[:, :], xt[:, :], AF.Square, [xt], [sq, sta], accum_out=sta[:, 0:1])
                        kb.ts("dve", sta[:, 1:2], sta[:, 0:1], 1.0 / D, EPS, ALU.mult, ALU.add, [sta], [sta])
                        kb.act(sta[:, 2:3], sta[:, 1:2], AF.Sqrt, [sta], [sta])
                        kb.op("dve", lambda e: e.reciprocal(out=sta[:, 3:4], in_=sta[:, 2:3]), [sta], [sta])
                        kb.stt(xt[:, :], xt[:, :], sta[:, 3:4], A[:, :], ALU.mult, ALU.mult, [xt, sta, A], [xt])
                        if router is None:
                            kb.tt("pool", hb[:, :], xt[:, :], Bt[:, :], ALU.add, [xt, Bt], [hb])
                        else:
                            kb.tt("pool", xt[:, :], xt[:, :], Bt[:, :], ALU.add, [xt, Bt], [xt])
                            kb.cp("act", hb[:, :], xt[:, :], [xt], [hb])
                        half = t % 2
                        if half == 0:
                            sg = stg[sti % 2]
                            sti += 1
                        for c0 in range(0, KC, 8):
                            n = min(8, KC - c0)
                            pt = self.next_ptr()
                            for c in range(n):
                                kb.tr(pt[:, c * 128:(c + 1) * 128], hb[:, (c0 + c) * 128:(c0 + c + 1) * 128], self.identb[:, :],
                                      [hb, self.identb], [pt], sig=(c == n - 1))
                            kb.cp("act" if (c0 // 8) % 2 == 0 else "dve",
                                  sg[:, c0:c0 + n, half * 128:(half + 1) * 128],
                                  pt[:, 0:n * 128].rearrange("p (c t) -> p c t", c=n), [pt], [sg])
                        if half == 1 or t == ntiles - 1:
                            wdt = (half + 1) * 128
                            sb0 = s0 + (t - half) * 128
                            kb.dma("sp", self.hxT.ap()[:, :, sb0:sb0 + wdt].rearrange("c p t -> p c t"),
                                   sg[:, :, 0:wdt], reads=[sg], writes=[self.hxT])
                        if router is not None:
                            for c0 in range(0, KC, 4):
                                n = min(4, KC - c0)
                                ps = self.next_pm()
                                for c in range(n):
                                    kb.tr(ps[:, c * 128:(c + 1) * 128], xt[:, (c0 + c) * 128:(c0 + c + 1) * 128], self.ident[:, :],
                                          [xt, self.ident], [ps], sig=(c == n - 1))
                                kb.cp("dve", h32T[:, c0:c0 + n, :], ps[:, 0:n * 128].rearrange("p (c t) -> p c t", c=n), [ps], [h32T])
                            ps = self.next_pm()
                            for kc in range(KC):
                                kb.mm(ps[:, 0:NE], h32T[:, kc, :], wr[:, kc, :], kc == 0, kc == KC - 1, [h32T, wr], [ps])
                            r = rs[it % 2]
                            kb.op("dve", lambda e: e.reduce_max(out=r[:, 0:1], in_=ps[:, 0:NE], axis=AX.X), [ps], [r])
                            kb.ts("dve", r[:, 1:2], r[:, 0:1], -1.0, None, ALU.mult, None, [r], [r])
                            kb.act(r[:, 16:32], ps[:, 0:NE], AF.Exp, [ps, r], [r], bias=r[:, 1:2], accum_out=r[:, 2:3])
                            kb.op("dve", lambda e: e.reciprocal(out=r[:, 3:4], in_=r[:, 2:3]), [r], [r])
                            kb.ts("dve", r[:, 32:48], r[:, 16:32], r[:, 3:4], None, ALU.mult, None, [r], [r])
                            ps2 = self.next_pm()
                            kb.tr(ps2[0:NE, 0:128], r[:, 32:48], self.ident[:, :], [r, self.ident], [ps2])
                            sa = s0 + t * 128
                            kb.cp("act", router["affT"][:, sa:sa + 128], ps2[0:NE, 0:128], [ps2], [router["affT"]])
            kb.barrier()

    def load_xT(self, xT, srcs, s0, G):
        kb = self.kb
        for (src, c0, nch, d0) in srcs:
            kb.dma("sp", xT[:, d0:d0 + nch, 0:G], src.ap()[c0:c0 + nch, :, s0:s0 + G].rearrange("c p t -> p c t"),
                   reads=[src], writes=[xT])

    def gemm(self, xT, KCn, G, blocks, wts):
        kb = self.kb
        nt = (G + 127) // 128

        def issue_loads(bi):
            wt = wts[bi % 2]
            for (c0, ncols, sap, sT) in blocks[bi]["loads"]:
                import os
                if os.environ.get("WSPLIT", "0") == "1":
                    for kc in range(KCn - int(os.environ.get('LESSROWS', '0'))):
                        kb.dma("sp", wt[:, kc, c0:c0 + ncols], sap[:, kc, :], reads=[sT], writes=[wt])
                else:
                    kb.dma(os.environ.get("WQ", "sp"), wt[:, 0:KCn, c0:c0 + ncols], sap, reads=[sT], writes=[wt])

        issue_loads(0)
        for bi, blk in enumerate(blocks):
            if bi + 1 < len(blocks):
                issue_loads(bi + 1)
            wt = wts[bi % 2]
            import os
            for (mode, c0, n, cb) in blk["items"][:(int(os.environ.get('CUTI', '99')) if bi > 0 else 99)]:
                if mode == "tok":
                    for t in range(nt):
                        ps = self.next_pm()
                        for kc in range(KCn):
                            kb.mm(ps[:, 0:n], xT[:, kc, t * 128:(t + 1) * 128], wt[:, kc, c0:c0 + n],
                                  kc == 0, kc == KCn - 1, [xT, wt], [ps], sig=(kc == KCn - 1))
                        cb(t, ps)
                else:
                    ps = self.next_pm()
                    for kc in range(KCn):
                        kb.mm(ps[0:n, 0:G], wt[:, kc, c0:c0 + n], xT[:, kc, 0:G],
                              kc == 0, kc == KCn - 1, [xT, wt], [ps], sig=(kc == KCn - 1))
                    cb(ps)

    def wslice(self, name, l, KCn, c0, ncols, r0=0):
        g = self.W(name, l)
        return g.ap()[r0:r0 + KCn * 128, c0:c0 + ncols].rearrange("(c p) n -> p c n", p=128), g

    def rope_epi(self, ps, M, G, rot, cos, sin, tmp32, tmpb, out_sb, scale_bc=None):
        kb = self.kb
        if scale_bc is not None:
            kb.tt("dve", tmp32[0:M, 0:G], ps[0:M, 0:G], scale_bc[0][0:M, 0:G], ALU.mult, [ps, scale_bc[1]], [tmp32])
        else:
            kb.cp("act", tmp32[0:M, 0:G], ps[0:M, 0:G], [ps], [tmp32])
        p2 = self.next_pm()
        kb.mm(p2[0:M, 0:G], rot[0:M, 0:M], tmp32[0:M, 0:G], True, True, [rot, tmp32], [p2])
        kb.tt("dve", tmpb[0:M, 0:G], p2[0:M, 0:G], sin[0], ALU.mult, [p2, sin[1]], [tmpb])
        kb.tt("pool", tmp32[0:M, 0:G], tmp32[0:M, 0:G], cos[0], ALU.mult, [tmp32, cos[1]], [tmp32])
        kb.tt("pool", out_sb[0:M, 0:G], tmp32[0:M, 0:G], tmpb[0:M, 0:G], ALU.add, [tmp32, tmpb], [out_sb])

    def phase_halo(self, xsrc):
        kb, D, TL = self.kb, self.D, self.TL
        with contextlib.ExitStack() as st:
            eb = [kb.T_sb(st, f"eb{j}", [128, D], F32) for j in range(2)]
            kb.dma("sp", eb[0][:, :], xsrc.ap()[0:128, :], reads=[xsrc], writes=[eb[0]])
            kb.dma("sp", eb[1][:, :], xsrc.ap()[TL - 128:TL, :], reads=[xsrc], writes=[eb[1]])
            kb.dma("pool", self.edge_loc.ap()[0:128, :], eb[0][:, :], reads=[eb[0]], writes=[self.edge_loc])
            kb.dma("pool", self.edge_loc.ap()[128:256, :], eb[1][:, :], reads=[eb[1]], writes=[self.edge_loc])
            import os
            CUT = int(os.environ.get('CUT', '99'))
            if CUT >= 1:
                kb.allgather(self.edge_loc, self.edge_all)
            oh = kb.T_sb(st, "oh", [128, 16], F32)
            kb.dma("sp", oh[:, 0:8], self.c_ohp.ap(), reads=[self.c_ohp], writes=[oh])
            kb.dma("sp", oh[:, 8:16], self.c_ohn.ap(), reads=[self.c_ohn], writes=[oh])
            acc = [kb.T_sb(st, f"hacc{j}", [128, D], F32) for j in range(2)]
            ed = [kb.T_sb(st, f"hed{j}", [128, D], F32) for j in range(2)]
            it = 0
            for side in range(2 if CUT >= 2 else 0):
                a = acc[side]
                for r in range(NCORE):
                    e = ed[it % 2]
                    it += 1
                    ro = r * 256 + (128 if side == 0 else 0)
                    kb.dma("sp", e[:, :], self.edge_all.ap()[ro:ro + 128, :], reads=[self.edge_all], writes=[e])
                    sc = oh[:, side * 8 + r:side * 8 + r + 1]
                    if r == 0:
                        kb.ts("dve", a[:, :], e[:, :], sc, None, ALU.mult, None, [e, oh], [a])
                    else:
                        kb.stt(a[:, :], e[:, :], sc, a[:, :], ALU.mult, ALU.add, [e, oh, a], [a])
                kb.dma("sp", self.xhalo.ap()[side * 128:(side + 1) * 128, :], a[:, :], reads=[a], writes=[self.xhalo])
            kb.barrier()

    def phase_even_in(self, l, need_ctx):
        kb, D, KC, TL = self.kb, self.D, self.KC, self.TL
        i = l // 2
        with contextlib.ExitStack() as st:
            xT = kb.T_sb(st, "xT", [128, KC, 512], BF16)
            wts = [kb.T_sb(st, f"wt{j}", [128, KC, 512], BF16) for j in range(2)]
            cos = kb.T_sb(st, "cos", [128, 512], F32)
            sin = kb.T_sb(st, "sin", [128, 512], F32)
            tmk = kb.T_sb(st, "tmk", [128, 256], F32)
            kb.dma("sp", tmk[:, :], self.c_tmask.ap()[0, :].partition_broadcast(128), reads=[self.c_tmask], writes=[tmk])
            t32 = [kb.T_sb(st, f"t32_{j}", [128, 512], F32) for j in range(2)]
            tb = [kb.T_sb(st, f"tb_{j}", [128, 512], F32) for j in range(2)]
            ob = [kb.T_sb(st, f"ob_{j}", [128, 512], BF16) for j in range(3)]
            sg = [kb.T_sb(st, f"sg_{j}", [128, 512], F32) for j in range(2)]
            uo = [kb.T_sb(st, f"uo_{j}", [128, 512], F32) for j in range(2)]
            cnt = [0]
            groups = [(g, "own") for g in self.own_groups()] + [(self.halo_group(), "halo"), (self.ctx_group(), "ctx")]
            for (s0, G), kind in groups:
                self.load_xT(xT, [(self.hxT, 0, KC, 0)], s0, G)
                lat = kind != "ctx"
                import os
                if int(os.environ.get('NOROPE', '0')):
                    lat = False
                if lat:
                    kb.dma("sp", cos[:, 0:G], self.c_cosE.ap()[:, s0:s0 + G], reads=[self.c_cosE], writes=[cos])
                    kb.dma("sp", sin[:, 0:G], self.c_sinE.ap()[:, s0:s0 + G], reads=[self.c_sinE], writes=[sin])
                blocks = []

                def qk_cb(dst, hidx):
                    def cb(ps):
                        cnt[0] += 1
                        o = ob[cnt[0] % 3]
                        if lat:
                            self.rope_epi(ps, 128, G, self.rotE, (cos[:, 0:G], cos), (sin[:, 0:G], sin),
                                          t32[cnt[0] % 2], tb[cnt[0] % 2], o)
                        else:
                            kb.cp("act", o[:, 0:G], ps[:, 0:G], [ps], [o])
                        kb.dma("pool", dst.ap()[hidx, :, s0:s0 + G], o[:, 0:G], reads=[o], writes=[dst])
                    return cb

                if kind != "halo":
                    for b in range(4):
                        sap, sT = self.wslice("ev_w_in", i, KC, (0 if int(os.environ.get("SAMECOL", "0")) else b) * 512, 512)
                        blocks.append(dict(loads=[(0, 512, sap, sT)],
                                           items=[("feat", j * 128, 128, qk_cb(self.qT, b * 4 + j)) for j in range(4)]))
                sap, sT = self.wslice("ev_w_in", i, KC, 2048, 512)
                blocks.append(dict(loads=[(0, 512, sap, sT)],
                                   items=[("feat", j * 128, 128, qk_cb(self.kT, j)) for j in range(4)]))

                def v_cb(t, ps):
                    cnt[0] += 1
                    o = ob[cnt[0] % 3]
                    kb.cp("act", o[:, :], ps[:, :], [ps], [o])
                    kb.dma("pool", self.vE.ap()[s0 + t * 128:s0 + (t + 1) * 128, :], o[:, :], reads=[o], writes=[self.vE])
                sap, sT = self.wslice("ev_w_in", i, KC, 2560, 512)
                blocks.append(dict(loads=[(0, 512, sap, sT)], items=[("tok", 0, 512, v_cb)]))

                def g_cb(slot):
                    def cb(ps):
                        kb.act(sg[slot][:, 0:G], ps[:, 0:G], AF.Sigmoid, [ps], [sg[slot]])
                    return cb

                def a_cb(slot, cc):
                    def cb(ps):
                        cnt[0] += 1
                        u = uo[cnt[0] % 2]
                        kb.tt("dve", u[:, 0:G], ps[:, 0:G], sg[slot][:, 0:G], ALU.mult, [ps, sg[slot]], [u])
                        if kind == "halo":
                            kb.tt("pool", u[:, 0:G], u[:, 0:G], tmk[:, 0:G], ALU.mult, [u, tmk], [u])
                        kb.dma("pool", self.uT.ap()[cc, :, s0:s0 + G], u[:, 0:G], reads=[u], writes=[self.uT])
                    return cb
                for b in range(8):
                    sa, sTa = self.wslice("ev_w_in", i, KC, 3072 + b * 256, 256)
                    sgp, sTg = self.wslice("ev_w_in", i, KC, 5120 + b * 256, 256)
                    blocks.append(dict(loads=[(0, 256, sa, sTa), (256, 256, sgp, sTg)],
                                       items=[("feat", 256, 128, g_cb(0)), ("feat", 0, 128, a_cb(0, b * 2)),
                                              ("feat", 384, 128, g_cb(1)), ("feat", 128, 128, a_cb(1, b * 2 + 1))]))
                import os
                blocks = blocks[:int(os.environ.get('CUTB', '99'))]
                self.gemm(xT, KC, G, blocks, wts)
                if int(os.environ.get('CUTG', '0')):
                    break
            kb.barrier()

    def phase_even_attn(self, l, need_ctx):
        kb, TL, NT, TS = self.kb, self.TL, self.NT, self.TS
        i = l // 2
        NST = TS // 128
        scale = float(A_DH) ** -0.5
        with contextlib.ExitStack() as st:
            kTs = kb.T_sb(st, "kTs", [128, A_KV, TS], BF16)
            V = kb.T_sb(st, "Vaug", [128, NST, A_KV, 129], BF16)
            kb.dma("sp", kTs[:, :, :], self.kT.ap().rearrange("h p t -> p h t"), reads=[self.kT], writes=[kTs])
            kb.memset("dve", V[:, :, :, 128:129], 1.0, [V])
            for k0 in range(NST):
                kb.dma("sp", V[:, k0, :, 0:128], self.vE.ap()[k0 * 128:(k0 + 1) * 128, :].rearrange("p (h d) -> p h d", h=A_KV),
                       reads=[self.vE], writes=[V])
            triA = kb.T_sb(st, "triA", [128, 512], F32)
            triB = kb.T_sb(st, "triB", [128, 512], F32)
            kb.dma("sp", triA[:, :], self.c_triA.ap(), reads=[self.c_triA], writes=[triA])
            kb.dma("sp", triB[:, :], self.c_triB.ap(), reads=[self.c_triB], writes=[triB])
            km = kb.T_sb(st, "km", [128, 2], F32)
            kb.dma("sp", km[:, :], self.c_kmask.ap(), reads=[self.c_kmask], writes=[km])
            esk = kb.T_sb(st, "esk", [128, A_HEADS], F32)
            kb.dma("sp", esk[:, :], self.sink.ap()[i, :].partition_broadcast(128), reads=[self.sink], writes=[esk])
            kb.act(esk[:, :], esk[:, :], AF.Exp, [esk], [esk])
            Qt = [kb.T_sb(st, f"Qt{j}", [128, A_HEADS * 128], BF16) for j in range(2)]
            pT = [kb.T_sb(st, f"pT{j}", [128, 512], BF16) for j in range(6)]
            Ot = [kb.T_sb(st, f"Ot{j}", [128, A_HEADS * 128], BF16) for j in range(2)]
            rr = [kb.T_sb(st, f"rr{j}", [128, 2], F32) for j in range(4)]
            stg = [kb.T_sb(st, f"ostg{j}", [128, A_HEADS, 128], BF16) for j in range(2)]
            pi = 0
            ri = 0
            qtiles = [("own", n) for n in range(NT)]
            if need_ctx:
                qtiles += [("ctx", 0), ("ctx", 1)]
            for qi, (kind, n) in enumerate(qtiles):
                Q, O = Qt[qi % 2], Ot[qi % 2]
                if kind == "own":
                    sq0 = n * 128
                    prev = NT if n == 0 else n - 1
                    nxt = NT + 1 if n == NT - 1 else n + 1
                    keys = [(prev, "A", 0 if n == 0 else None), (n, None, None),
                            (nxt, "B", 1 if n == NT - 1 else None), (NT + 2, None, None), (NT + 3, None, None)]
                else:
                    sq0 = TL + 256 + n * 128
                    keys = [(NT + 2, None, None), (NT + 3, None, None)]
                kb.dma("sp", Q[:, :].rearrange("p (h t) -> p h t", h=A_HEADS),
                       self.qT.ap()[:, :, sq0:sq0 + 128].rearrange("h p t -> p h t"), reads=[self.qT], writes=[Q])
                for g in range(A_KV):
                    pts = []
                    for (kt, msk, kmc) in keys:
                        ps = self.next_po()
                        kb.mm(ps[:, :], kTs[:, g, kt * 128:(kt + 1) * 128], Q[:, g * 512:(g + 1) * 512], True, True, [kTs, Q], [ps])
                        p = pT[pi % 6]
                        pi += 1
                        kb.act(p[:, :], ps[:, :], AF.Exp, [ps], [p], scale=scale)
                        if msk is not None:
                            tri = triA if msk == "A" else triB
                            sc = km[:, kmc:kmc + 1] if kmc is not None else 1.0
                            rd = [p, tri] + ([km] if kmc is not None else [])
                            kb.stt(p[:, :], p[:, :], sc, tri[:, :], ALU.mult, ALU.mult, rd, [p])
                        pts.append((p, kt))
                    for j in range(4):
                        h = g * 4 + j
                        po_ = self.next_pm()
                        for ki, (p, kt) in enumerate(pts):
                            kb.mm(po_[:, 0:129], p[:, j * 128:(j + 1) * 128], V[:, kt, g, :], ki == 0, ki == len(pts) - 1, [p, V], [po_])
                        r = rr[ri % 4]
                        ri += 1
                        kb.ts("dve", r[:, 0:1], po_[:, 128:129], esk[:, h:h + 1], None, ALU.add, None, [po_, esk], [r])
                        kb.op("dve", lambda e: e.reciprocal(out=r[:, 1:2], in_=r[:, 0:1]), [r], [r])
                        kb.act(O[:, h * 128:(h + 1) * 128], po_[:, 0:128], AF.Copy, [po_, r], [O], scale=r[:, 1:2])
                sg = stg[qi % 2]
                for c0 in range(0, A_HEADS, 8):
                    pt = self.next_ptr()
                    for c in range(8):
                        kb.tr(pt[:, c * 128:(c + 1) * 128], O[:, (c0 + c) * 128:(c0 + c + 1) * 128], self.identb[:, :],
                              [O, self.identb], [pt], sig=(c == 7))
                    kb.cp("dve", sg[:, c0:c0 + 8, :], pt[:, :].rearrange("p (c t) -> p c t", c=8), [pt], [sg])
                kb.dma("pool", self.mixT.ap()[0:A_HEADS, :, sq0:sq0 + 128].rearrange("c p t -> p c t"), sg[:, :, :],
                       reads=[sg], writes=[self.mixT])
            kb.barrier()

    def phase_even_conv(self, l, need_ctx):
        kb, TL = self.kb, self.TL
        i = l // 2
        with contextlib.ExitStack() as st:
            dw = kb.T_sb(st, "dw", [128, 16, B_W], F32)
            lng = kb.T_sb(st, "lng", [128, 16], F32)
            lnb = kb.T_sb(st, "lnb", [128, 16], F32)
            kb.dma("sp", dw[:, :, :], self.dwT.ap()[i], reads=[self.dwT], writes=[dw])
            kb.dma("sp", lng[:, :], self.lngT.ap()[i], reads=[self.lngT], writes=[lng])
            kb.dma("sp", lnb[:, :], self.lnbT.ap()[i], reads=[self.lnbT], writes=[lnb])
            uin = [kb.T_sb(st, f"uin{j}", [128, 512 + 30], F32) for j in range(2)]
            z = kb.T_sb(st, "z", [128, 16, 512], F32)
            zsq = [kb.T_sb(st, f"zsq{j}", [128, 512], F32) for j in range(2)]
            mean = kb.T_sb(st, "mean", [128, 512], F32)
            m2 = kb.T_sb(st, "m2", [128, 512], F32)
            rstd = kb.T_sb(st, "rstd", [128, 512], F32)
            zn = [kb.T_sb(st, f"zn{j}", [128, 512], F32) for j in range(2)]
            co = [kb.T_sb(st, f"co{j}", [128, 512], BF16) for j in range(2)]
            groups = [(g, "own") for g in self.own_groups()]
            if need_ctx:
                groups.append((self.ctx_group(), "ctx"))
            it = 0
            for (s0, G), kind in groups:
                pz, pq = self.po[0], self.po[1]
                for cc in range(16):
                    u = uin[it % 2]
                    zq = zsq[it % 2]
                    it += 1
                    src = self.uT.ap()[cc]
                    kb.dma("sp", u[:, 15:15 + G], src[:, s0:s0 + G], reads=[self.uT], writes=[u])
                    if kind == "ctx":
                        kb.memset("pool", u[:, 0:15], 0.0, [u])
                        kb.memset("pool", u[:, 15 + G:30 + G], 0.0, [u])
                    else:
                        l0 = s0 - 15 if s0 > 0 else TL + 128 - 15
                        r0 = s0 + G if s0 + G < TL else TL + 128
                        kb.dma("sp", u[:, 0:15], src[:, l0:l0 + 15], reads=[self.uT], writes=[u])
                        kb.dma("sp", u[:, 15 + G:30 + G], src[:, r0:r0 + 15], reads=[self.uT], writes=[u])
                    kb.ts("dve", z[:, cc, 0:G], u[:, 0:G], dw[:, cc, 0:1], None, ALU.mult, None, [u, dw], [z])
                    for j in range(1, B_W):
                        kb.stt(z[:, cc, 0:G], u[:, j:j + G], dw[:, cc, j:j + 1], z[:, cc, 0:G], ALU.mult, ALU.add, [u, dw, z], [z])
                    kb.act(zq[:, 0:G], z[:, cc, 0:G], AF.Square, [z], [zq])
                    kb.mm(pz[:, 0:G], self.onesf[:, :], z[:, cc, 0:G], cc == 0, cc == 15, [self.onesf, z], [pz])
                    kb.mm(pq[:, 0:G], self.onesf[:, :], zq[:, 0:G], cc == 0, cc == 15, [self.onesf, zq], [pq])
                kb.act(mean[:, 0:G], pz[:, 0:G], AF.Copy, [pz], [mean], scale=1.0 / B_CH)
                kb.tt("dve", m2[:, 0:G], mean[:, 0:G], mean[:, 0:G], ALU.mult, [mean], [m2])
                kb.stt(rstd[:, 0:G], pq[:, 0:G], 1.0 / B_CH, m2[:, 0:G], ALU.mult, ALU.subtract, [pq, m2], [rstd])
                kb.ts("dve", rstd[:, 0:G], rstd[:, 0:G], EPS, None, ALU.add, None, [rstd], [rstd])
                kb.act(rstd[:, 0:G], rstd[:, 0:G], AF.Sqrt, [rstd], [rstd])
                kb.op("dve", lambda e: e.reciprocal(out=rstd[:, 0:G], in_=rstd[:, 0:G]), [rstd], [rstd])
                for cc in range(16):
                    a, o = zn[cc % 2], co[cc % 2]
                    kb.tt("dve", a[:, 0:G], z[:, cc, 0:G], mean[:, 0:G], ALU.subtract, [z, mean], [a])
                    kb.tt("pool", a[:, 0:G], a[:, 0:G], rstd[:, 0:G], ALU.mult, [a, rstd], [a])
                    kb.act(o[:, 0:G], a[:, 0:G], AF.Silu, [a, lng, lnb], [o], scale=lng[:, cc:cc + 1], bias=lnb[:, cc:cc + 1])
                    kb.dma("pool", self.mixT.ap()[16 + cc, :, s0:s0 + G], o[:, 0:G], reads=[o], writes=[self.mixT])
            kb.barrier()

    def phase_out_gemm(self, wname, wl, KCn, src, l, gate_j, xin, xout, yin, yout, need_ctx, nw):
        kb, D, TL = self.kb, self.D, self.TL
        nw = min(nw, D)
        with contextlib.ExitStack() as st:
            xT = kb.T_sb(st, "xT", [128, KCn, 512], BF16)
            wts = [kb.T_sb(st, f"wt{j}", [128, KCn, nw], BF16) for j in range(2)]
            gate = kb.T_sb(st, "gate", [128, D], F32)
            xts = [kb.T_sb(st, f"xr{j}", [128, nw], F32) for j in range(3)]
            tmps = [kb.T_sb(st, f"xm{j}", [128, nw], F32) for j in range(3)]
            cnt = [0]
            groups = [(g, 0) for g in self.own_groups()]
            if need_ctx:
                groups.append((self.ctx_group(), 1))
            lastw = None
            for (s0, G), which in groups:
                if which != lastw:
                    self.load_bc("sp", gate, self.mod_vec(which, l, gate_j), [self.mods_all])
                    lastw = which
                self.load_xT(xT, [(src, 0, KCn, 0)], s0, G)
                xi, xo = (xin, xout) if which == 0 else (yin, yout)
                rbase = s0 if which == 0 else s0 - (TL + 256)
                blocks = []
                for n0 in range(0, D, nw):
                    def cb(t, ps, n0=n0):
                        cnt[0] += 1
                        xt, tm = xts[cnt[0] % 3], tmps[cnt[0] % 3]
                        r0 = rbase + t * 128
                        kb.dma("sp", xt[:, :], xi.ap()[r0:r0 + 128, n0:n0 + nw], reads=[xi], writes=[xt])
                        kb.tt("dve", tm[:, :], ps[:, 0:nw], gate[:, n0:n0 + nw], ALU.mult, [ps, gate], [tm])
                        kb.tt("pool", tm[:, :], tm[:, :], xt[:, :], ALU.add, [tm, xt], [tm])
                        kb.dma("pool", xo.ap()[r0:r0 + 128, n0:n0 + nw], tm[:, :], reads=[tm], writes=[xo])
                    sap, sT = self.wslice(wname, wl, KCn, n0, nw)
                    blocks.append(dict(loads=[(0, nw, sap, sT)], items=[("tok", 0, nw, cb)]))
                self.gemm(xT, KCn, G, blocks, wts)
            kb.barrier()

    def phase_odd_dn(self, l, need_ctx):
        kb, D, KC, TL = self.kb, self.D, self.KC, self.TL
        i = l // 2
        with contextlib.ExitStack() as st:
            xT = kb.T_sb(st, "xT", [128, KC, 512], BF16)
            wts = [kb.T_sb(st, f"wt{j}", [128, KC, 512], BF16) for j in range(2)]
            qng = kb.T_sb(st, "qng", [128, 8], F32)
            kvng = kb.T_sb(st, "kvng", [128, 4], F32)
            kb.dma("sp", qng[:, :], self.qngT.ap()[i], reads=[self.qngT], writes=[qng])
            kb.dma("sp", kvng[:, :], self.kvngT.ap()[i], reads=[self.kvngT], writes=[kvng])
            cos = kb.T_sb(st, "cos", [64, 512], F32)
            sin = kb.T_sb(st, "sin", [64, 512], F32)
            sqb = [kb.T_sb(st, f"sqb{j}", [128, 512], BF16) for j in range(2)]
            ob = [kb.T_sb(st, f"ob{j}", [128, 512], BF16) for j in range(3)]
            row = [kb.T_sb(st, f"row{j}", [1, 512], F32) for j in range(2)]
            t32 = kb.T_sb(st, "t32", [128, 512], F32)
            tb = kb.T_sb(st, "tb", [128, 512], F32)
            cnt = [0]
            groups = [(g, "own") for g in self.own_groups()] + [(self.ctx_group(), "ctx")]
            for (s0, G), kind in groups:
                self.load_xT(xT, [(self.hxT, 0, KC, 0)], s0, G)
                if kind == "own":
                    kb.dma("sp", cos[:, 0:G], self.c_cosM.ap()[:, s0:s0 + G], reads=[self.c_cosM], writes=[cos])
                    kb.dma("sp", sin[:, 0:G], self.c_sinM.ap()[:, s0:s0 + G], reads=[self.c_sinM], writes=[sin])

                def lat_cb(c, nch, gvec, dst, acc, rdst, ridx, dim):
                    def cb(ps):
                        cnt[0] += 1
                        sq, o = sqb[cnt[0] % 2], ob[cnt[0] % 3]
                        kb.act(sq[:, 0:G], ps[:, 0:G], AF.Square, [ps], [sq])
                        kb.mm(acc[:, 0:G], self.onesb[:, :], sq[:, 0:G], c == 0, c == nch - 1, [self.onesb, sq], [acc])
                        kb.act(o[:, 0:G], ps[:, 0:G], AF.Copy, [ps, gvec], [o], scale=gvec[:, c:c + 1])
                        kb.dma("pool", dst.ap()[c, :, s0:s0 + G], o[:, 0:G], reads=[o], writes=[dst])
                        if c == nch - 1:
                            rw = row[ridx]
                            kb.ts("dve", rw[:, 0:G], acc[0:1, 0:G], 1.0 / dim, EPS, ALU.mult, ALU.add, [acc], [rw])
                            kb.act(rw[:, 0:G], rw[:, 0:G], AF.Sqrt, [rw], [rw])
                            kb.op("dve", lambda e: e.reciprocal(out=rw[:, 0:G], in_=rw[:, 0:G]), [rw], [rw])
                            kb.dma("pool", rdst.ap()[:, s0:s0 + G], rw[:, 0:G], reads=[rw], writes=[rdst])
                    return cb

                def kr_cb(ps):
                    cnt[0] += 1
                    o = ob[cnt[0] % 3]
                    if kind == "own":
                        self.rope_epi(ps, 64, G, self.rotM, (cos[:, 0:G], cos), (sin[:, 0:G], sin), t32, tb, o)
                        kb.dma("pool", self.kr_loc.ap()[:, s0:s0 + G], o[0:64, 0:G], reads=[o], writes=[self.kr_loc])
                    else:
                        kb.cp("act", o[0:64, 0:G], ps[0:64, 0:G], [ps], [o])
                        kb.dma("pool", self.kr_ctx.ap()[:, 0:G], o[0:64, 0:G], reads=[o], writes=[self.kr_ctx])
                blocks = []
                for b in range(2):
                    sap, sT = self.wslice("od_w_dn", i, KC, b * 512, 512)
                    blocks.append(dict(loads=[(0, 512, sap, sT)],
                                       items=[("feat", j * 128, 128, lat_cb(b * 4 + j, 8, qng, self.dqT, self.po[0], self.rstdq, 0, C_QR))
                                              for j in range(4)]))
                sap, sT = self.wslice("od_w_dn", i, KC, 1024, 512)
                blocks.append(dict(loads=[(0, 512, sap, sT)],
                                   items=[("feat", j * 128, 128, lat_cb(j, 4, kvng, self.ckvT, self.po[1], self.rstdkv, 1, C_KVR))
                                          for j in range(4)]))
                sap, sT = self.wslice("od_w_dn", i, KC, 1536, 64)
                blocks.append(dict(loads=[(0, 64, sap, sT)], items=[("feat", 0, 64, kr_cb)]))
                self.gemm(xT, KC, G, blocks, wts)
            kb.barrier()

    def phase_odd_qkv(self, l, need_ctx):
        kb, TL = self.kb, self.TL
        i = l // 2
        with contextlib.ExitStack() as st:
            xq = kb.T_sb(st, "xq", [128, 8, 512], BF16)
            xk = kb.T_sb(st, "xk", [128, 4, 512], BF16)
            wq = [kb.T_sb(st, f"wq{j}", [128, 8, 768], BF16) for j in range(2)]
            wk = [kb.T_sb(st, f"wk{j}", [128, 4, 512], BF16) for j in range(2)]
            rq = kb.T_sb(st, "rq", [128, 512], F32)
            rk = kb.T_sb(st, "rk", [128, 512], F32)
            rkt = kb.T_sb(st, "rkt", [128, 4], F32)
            cos = kb.T_sb(st, "cos", [64, 512], F32)
            sin = kb.T_sb(st, "sin", [64, 512], F32)
            ob = [kb.T_sb(st, f"ob{j}", [128, 512], BF16) for j in range(3)]
            ov = [kb.T_sb(st, f"ov{j}", [128, 128], BF16) for j in range(3)]
            t32 = kb.T_sb(st, "t32", [128, 512], F32)
            tb = kb.T_sb(st, "tb", [128, 512], F32)
            cnt = [0]
            groups = [(g, "own") for g in self.own_groups()] + [(self.ctx_group(), "ctx")]
            for (s0, G), kind in groups:
                own = kind == "own"
                kb.dma("sp", rk[:, 0:G], self.rstdkv.ap()[0, s0:s0 + G].partition_broadcast(128), reads=[self.rstdkv], writes=[rk])
                with self.nc.allow_non_contiguous_dma(reason="tiny per-token scalars"):
                    kb.dma("sp", rkt[:, 0:G // 128], self.rstdkv.ap()[0, s0:s0 + G].rearrange("(t p) -> p t", p=128),
                           reads=[self.rstdkv], writes=[rkt])
                if own or need_ctx:
                    kb.dma("sp", rq[:, 0:G], self.rstdq.ap()[0, s0:s0 + G].partition_broadcast(128), reads=[self.rstdq], writes=[rq])
                    if own:
                        kb.dma("sp", cos[:, 0:G], self.c_cosM.ap()[:, s0:s0 + G], reads=[self.c_cosM], writes=[cos])
                        kb.dma("sp", sin[:, 0:G], self.c_sinM.ap()[:, s0:s0 + G], reads=[self.c_sinM], writes=[sin])
                    self.load_xT(xq, [(self.dqT, 0, 8, 0)], s0, G)

                    def qn_cb(h):
                        def cb(ps):
                            cnt[0] += 1
                            o = ob[cnt[0] % 3]
                            kb.tt("dve", o[:, 0:G], ps[:, 0:G], rq[:, 0:G], ALU.mult, [ps, rq], [o])
                            kb.dma("pool", self.qnT.ap()[h, :, s0:s0 + G], o[:, 0:G], reads=[o], writes=[self.qnT])
                        return cb

                    def qr_cb(h):
                        def cb(ps):
                            cnt[0] += 1
                            o = ob[cnt[0] % 3]
                            if own:
                                self.rope_epi(ps, 64, G, self.rotM, (cos[:, 0:G], cos), (sin[:, 0:G], sin), t32, tb, o,
                                              scale_bc=(rq, rq))
                            else:
                                kb.tt("dve", o[0:64, 0:G], ps[0:64, 0:G], rq[0:64, 0:G], ALU.mult, [ps, rq], [o])
                            kb.dma("pool", self.qrT.ap()[h, :, s0:s0 + G], o[0:64, 0:G], reads=[o], writes=[self.qrT])
                        return cb
                    blocks = []
                    for b in range(C_HEADS // 4):
                        sap, sT = self.wslice("od_w_uq", i, 8, b * 768, 768)
                        items = []
                        for j in range(4):
                            items.append(("feat", j * 192, 128, qn_cb(b * 4 + j)))
                            items.append(("feat", j * 192 + 128, 64, qr_cb(b * 4 + j)))
                        blocks.append(dict(loads=[(0, 768, sap, sT)], items=items))
                    self.gemm(xq, 8, G, blocks, wq)
                self.load_xT(xk, [(self.ckvT, 0, 4, 0)], s0, G)

                def kn_cb(h):
                    def cb(ps):
                        cnt[0] += 1
                        o = ob[cnt[0] % 3]
                        kb.tt("dve", o[:, 0:G], ps[:, 0:G], rk[:, 0:G], ALU.mult, [ps, rk], [o])
                        if own:
                            kb.dma("pool", self.kn_loc.ap()[h, :, s0:s0 + G], o[:, 0:G], reads=[o], writes=[self.kn_loc])
                        else:
                            kb.dma("pool", self.kn_ctx.ap()[h, :, 0:G], o[:, 0:G], reads=[o], writes=[self.kn_ctx])
                    return cb

                def v_cb(h):
                    def cb(t, ps):
                        cnt[0] += 1
                        o = ov[cnt[0] % 3]
                        kb.act(o[:, :], ps[:, 0:128], AF.Copy, [ps, rkt], [o], scale=rkt[:, t:t + 1])
                        if own:
                            r0 = s0 + t * 128
                            kb.dma("pool", self.v_loc.ap()[r0:r0 + 128, h * 128:(h + 1) * 128], o[:, :], reads=[o], writes=[self.v_loc])
                        else:
                            r0 = t * 128
                            kb.dma("pool", self.v_ctx.ap()[r0:r0 + 128, h * 128:(h + 1) * 128], o[:, :], reads=[o], writes=[self.v_ctx])
                    return cb
                blocks = []
                for b in range(C_HEADS // 2):
                    sap, sT = self.wslice("od_w_ukv", i, 4, b * 512, 512)
                    items = []
                    for j in range(2):
                        items.append(("feat", j * 256, 128, kn_cb(b * 2 + j)))
                        items.append(("tok", j * 256 + 128, 128, v_cb(b * 2 + j)))
                    blocks.append(dict(loads=[(0, 512, sap, sT)], items=items))
                self.gemm(xk, 4, G, blocks, wk)
            kb.allgather(self.kn_loc, self.kn_all)
            kb.allgather(self.v_loc, self.v_all)
            kb.allgather(self.kr_loc, self.kr_all)
            kb.barrier()

    def phase_mla_attn(self, l, need_ctx):
        kb, TL, SEQ = self.kb, self.TL, self.SEQ
        NK = SEQ + CT
        NKT = NK // 128
        scale = float(C_NOPE + C_ROPE) ** -0.5
        with contextlib.ExitStack() as st:
            kr = kb.T_sb(st, "kr", [64, NK], BF16)
            kb.dma("sp", kr[:, 0:SEQ].rearrange("p (r t) -> p r t", r=NCORE), self.kr_all.ap().rearrange("r p t -> p r t"),
                   reads=[self.kr_all], writes=[kr])
            kb.dma("sp", kr[:, SEQ:NK], self.kr_ctx.ap(), reads=[self.kr_ctx], writes=[kr])
            kn = [kb.T_sb(st, f"kn{j}", [128, NK], BF16) for j in range(2)]
            V = [kb.T_sb(st, f"V{j}", [128, NKT, 129], BF16) for j in range(2)]
            for j in range(2):
                kb.memset("dve", V[j][:, :, 128:129], 1.0, [V[j]])
            qn = [kb.T_sb(st, f"qn{j}", [128, TL + CT], BF16) for j in range(2)]
            qr = [kb.T_sb(st, f"qr{j}", [64, TL + CT], BF16) for j in range(2)]
            pT = [kb.T_sb(st, f"pT{j}", [128, 512], BF16) for j in range(8)]
            oo = [kb.T_sb(st, f"oo{j}", [128, 128], BF16) for j in range(4)]
            rr = [kb.T_sb(st, f"rr{j}", [128, 2], F32) for j in range(4)]
            pi = 0
            oi = 0
            qgroups = [(s0, G, list(range(NKT))) for (s0, G) in self.own_groups()]
            if need_ctx:
                qgroups.append((TL, CT, [NKT - 2, NKT - 1]))
            for h in range(C_HEADS):
                K_, V_, Qn, Qr = kn[h % 2], V[h % 2], qn[h % 2], qr[h % 2]
                kb.dma("sp", K_[:, 0:SEQ].rearrange("p (r t) -> p r t", r=NCORE), self.kn_all.ap()[:, h].rearrange("r p t -> p r t"),
                       reads=[self.kn_all], writes=[K_])
                kb.dma("sp", K_[:, SEQ:NK], self.kn_ctx.ap()[h], reads=[self.kn_ctx], writes=[K_])
                vsrc = self.v_all.ap()[:, h * 128:(h + 1) * 128].rearrange("(k p) d -> p k d", p=128)
                nlat = SEQ // 128
                for k0 in range(0, nlat, 32):
                    k1 = min(nlat, k0 + 32)
                    kb.dma("sp", V_[:, k0:k1, 0:128], vsrc[:, k0:k1, :], reads=[self.v_all], writes=[V_])
                kb.dma("sp", V_[:, nlat:NKT, 0:128], self.v_ctx.ap()[:, h * 128:(h + 1) * 128].rearrange("(k p) d -> p k d", p=128),
                       reads=[self.v_ctx], writes=[V_])
                kb.dma("sp", Qn[:, 0:TL], self.qnT.ap()[h, :, 0:TL], reads=[self.qnT], writes=[Qn])
                kb.dma("sp", Qr[:, 0:TL], self.qrT.ap()[h, :, 0:TL], reads=[self.qrT], writes=[Qr])
                if need_ctx:
                    kb.dma("sp", Qn[:, TL:TL + CT], self.qnT.ap()[h, :, TL + 256:TL + 512], reads=[self.qnT], writes=[Qn])
                    kb.dma("sp", Qr[:, TL:TL + CT], self.qrT.ap()[h, :, TL + 256:TL + 512], reads=[self.qrT], writes=[Qr])
                for (q0, G, kts) in qgroups:
                    nsub = G // 128
                    for ki, kt in enumerate(kts):
                        ps = self.next_po()
                        kb.mm(ps[:, 0:G], K_[:, kt * 128:(kt + 1) * 128], Qn[:, q0:q0 + G], True, False, [K_, Qn], [ps], sig=False)
                        kb.mm(ps[:, 0:G], kr[:, kt * 128:(kt + 1) * 128], Qr[:, q0:q0 + G], False, True, [kr, Qr], [ps])
                        p = pT[pi % 8]
                        pi += 1
                        kb.act(p[:, 0:G], ps[:, 0:G], AF.Exp, [ps], [p], scale=scale)
                        for j in range(nsub):
                            kb.mm(self.pm[j][:, 0:129], p[:, j * 128:(j + 1) * 128], V_[:, kt, :], ki == 0, ki == len(kts) - 1,
                                  [p, V_], [self.pm[j]], sig=(ki == len(kts) - 1 or j == nsub - 1))
                    for j in range(nsub):
                        r, o = rr[oi % 4], oo[oi % 4]
                        oi += 1
                        kb.op("dve", lambda e: e.reciprocal(out=r[:, 0:1], in_=self.pm[j][:, 128:129]), [self.pm[j]], [r])
                        kb.act(o[:, :], self.pm[j][:, 0:128], AF.Copy, [self.pm[j], r], [o], scale=r[:, 0:1])
                        srow = (q0 + j * 128) if q0 < TL else (TL + 256 + (q0 - TL) + j * 128)
                        kb.dma("pool", self.otok.ap()[srow:srow + 128, h * 128:(h + 1) * 128], o[:, :], reads=[o], writes=[self.otok])
            kb.barrier()

    def phase_otok_T(self, need_ctx):
        kb, TL = self.kb, self.TL
        with contextlib.ExitStack() as st:
            xt = [kb.T_sb(st, f"ot{j}", [128, 4096], BF16) for j in range(2)]
            stg = [kb.T_sb(st, f"otg{j}", [128, 32, 128], BF16) for j in range(2)]
            tiles = [n * 128 for n in range(self.NT)]
            if need_ctx:
                tiles += [TL + 256, TL + 384]
            for ti, s in enumerate(tiles):
                x, sg = xt[ti % 2], stg[ti % 2]
                kb.dma("sp", x[:, :], self.otok.ap()[s:s + 128, :], reads=[self.otok], writes=[x])
                for c0 in range(0, 32, 8):
                    pt = self.next_ptr()
                    for c in range(8):
                        kb.tr(pt[:, c * 128:(c + 1) * 128], x[:, (c0 + c) * 128:(c0 + c + 1) * 128], self.identb[:, :],
                              [x, self.identb], [pt], sig=(c == 7))
                    kb.cp("act" if (c0 // 8) % 2 else "dve", sg[:, c0:c0 + 8, :], pt[:, :].rearrange("p (c t) -> p c t", c=8), [pt], [sg])
                kb.dma("pool", self.mixT.ap()[:, :, s:s + 128].rearrange("c p t -> p c t"), sg[:, :, :], reads=[sg], writes=[self.mixT])
            kb.barrier()

    def bisect(self, st, A, P, X, cap, lo, tag):
        kb = self.kb
        junk = kb.T_sb(st, "bjunk" + tag, [P, X], F32)
        sm = kb.T_sb(st, "bsm" + tag, [P, 4], F32)
        kb.memset("dve", lo[:, :], 0.0, [lo])
        for k in range(30):
            w = 2.0 ** -(k + 1)
            kb.ts("dve", sm[:, 0:1], lo[:, 0:1], w, None, ALU.add, None, [lo], [sm])
            kb.ts("dve", junk[:, :], A, sm[:, 0:1], None, ALU.is_ge, ALU.add, [sm] + self._bis_reads, [junk, sm],
                  accum_out=sm[:, 1:2])
            ps = self.next_pm()
            kb.mm(ps[0:P, 0:1], self.m16[0:P, 0:P], sm[0:P, 1:2], True, True, [self.m16, sm], [ps])
            kb.ts("dve", sm[:, 2:3], ps[0:P, 0:1], cap - 0.5, w, ALU.is_ge, ALU.mult, [ps], [sm])
            kb.tt("dve", lo[:, 0:1], lo[:, 0:1], sm[:, 2:3], ALU.add, [lo, sm], [lo])

    def phase_route(self, st_outer, affT, need_ctx):
        kb, TL = self.kb, self.TL
        with contextlib.ExitStack() as st:
            kb.dma("pool", self.aff_loc.ap(), affT[:, 0:TL], reads=[affT], writes=[self.aff_loc])
            kb.allgather(self.aff_loc, self.aff_all)
            A = kb.T_sb(st, "affA", [128, TL], F32)
            kb.dma("sp", A[:, :], self.aff_all.ap(), reads=[self.aff_all], writes=[A])
            lo = kb.T_sb(st, "lo", [128, 1], F32)
            self._bis_reads = [A]
            self.bisect(st, A[:, :], 128, TL, float(TL), lo, "a")
            GTs = kb.T_sb(st, "GTs", [NE, self.TS], F32)
            kb.stt(GTs[:, 0:TL], affT[:, 0:TL], lo[0:NE, 0:1], affT[:, 0:TL], ALU.is_ge, ALU.mult, [affT, lo], [GTs])
            kb.dma("sp", self.GT.ap()[:, 0:TL], GTs[:, 0:TL], reads=[GTs], writes=[self.GT])
            if need_ctx:
                c0 = TL + 256
                lo2 = kb.T_sb(st, "lo2", [NE, 1], F32)
                self._bis_reads = [affT]
                self.bisect(st, affT[:, c0:c0 + CT], NE, CT, float(2 * CT // NE), lo2, "c")
                kb.stt(GTs[:, c0:c0 + CT], affT[:, c0:c0 + CT], lo2[:, 0:1], affT[:, c0:c0 + CT], ALU.is_ge, ALU.mult,
                       [affT, lo2], [GTs])
                kb.dma("sp", self.GT.ap()[:, c0:c0 + CT], GTs[:, c0:c0 + CT], reads=[GTs], writes=[self.GT])
            kb.barrier()

    def phase_moe_up(self, l, need_ctx):
        kb, D, KC, TL = self.kb, self.D, self.KC, self.TL
        with contextlib.ExitStack() as st:
            xT = kb.T_sb(st, "xT", [128, KC, 512], BF16)
            wts = [kb.T_sb(st, f"wt{j}", [128, KC, 768], BF16) for j in range(2)]
            GTg = kb.T_sb(st, "GTg", [NE, 512], F32)
            Gbc = kb.T_sb(st, "Gbc", [128, NE, 512], F32)
            sa = [kb.T_sb(st, f"sa{j}", [128, 512], F32) for j in range(2)]
            tu = [kb.T_sb(st, f"tu{j}", [128, 512], F32) for j in range(2)]
            ho = [kb.T_sb(st, f"ho{j}", [128, 512], BF16) for j in range(3)]
            cnt = [0]
            groups = list(self.own_groups())
            if need_ctx:
                groups.append(self.ctx_group())
            for (s0, G) in groups:
                self.load_xT(xT, [(self.hxT, 0, KC, 0)], s0, G)
                kb.dma("sp", GTg[:, 0:G], self.GT.ap()[:, s0:s0 + G], reads=[self.GT], writes=[GTg])
                for e in range(NE):
                    ps = self.next_pm()
                    kb.mm(ps[:, 0:G], self.sel[0:NE, e * 128:(e + 1) * 128], GTg[:, 0:G], True, True, [self.sel, GTg], [ps])
                    kb.cp("act", Gbc[:, e, 0:G], ps[:, 0:G], [ps], [Gbc])

                def a_cb(slot):
                    def cb(ps):
                        kb.act(sa[slot][:, 0:G], ps[:, 0:G], AF.Silu, [ps], [sa[slot]])
                    return cb

                def u_cb(slot, e, f):
                    def cb(ps):
                        cnt[0] += 1
                        t, o = tu[cnt[0] % 2], ho[cnt[0] % 3]
                        kb.tt("dve", t[:, 0:G], ps[:, 0:G], sa[slot][:, 0:G], ALU.mult, [ps, sa[slot]], [t])
                        kb.tt("pool", o[:, 0:G], t[:, 0:G], Gbc[:, e, 0:G], ALU.mult, [t, Gbc], [o])
                        kb.dma("pool", self.HT.ap()[e * 3 + f, :, s0:s0 + G], o[:, 0:G], reads=[o], writes=[self.HT])
                    return cb
                blocks = []
                for e in range(NE):
                    sg_, sTg = self.wslice("moe_g", l, KC, 0, EFF, r0=e * D)
                    su_, sTu = self.wslice("moe_u", l, KC, 0, EFF, r0=e * D)
                    items = []
                    for f in range(3):
                        items.append(("feat", f * 128, 128, a_cb(f % 2)))
                        items.append(("feat", EFF + f * 128, 128, u_cb(f % 2, e, f)))
                    blocks.append(dict(loads=[(0, EFF, sg_, sTg), (EFF, EFF, su_, sTu)], items=items))
                self.gemm(xT, KC, G, blocks, wts)
            kb.barrier()

    def phase_final(self, xsrc):
        kb, D, NT = self.kb, self.D, self.NT
        with contextlib.ExitStack() as st:
            g = kb.T_sb(st, "fg", [128, D], F32)
            self.load_bc("sp", g, self.norm_g.ap()[2 * DEPTH, :], [self.norm_g])
            xts = [kb.T_sb(st, f"fx{j}", [128, D], F32) for j in range(2)]
            sq = kb.T_sb(st, "fsq", [128, D], BF16)
            stat = [kb.T_sb(st, f"fstat{j}", [128, 4], F32) for j in range(2)]
            for t in range(NT):
                xt, sta = xts[t % 2], stat[t % 2]
                kb.dma("sp", xt[:, :], xsrc.ap()[t * 128:(t + 1) * 128, :], reads=[xsrc], writes=[xt])
                kb.act(sq[:, :], xt[:, :], AF.Square, [xt], [sq, sta], accum_out=sta[:, 0:1])
                kb.ts("dve", sta[:, 1:2], sta[:, 0:1], 1.0 / D, EPS, ALU.mult, ALU.add, [sta], [sta])
                kb.act(sta[:, 2:3], sta[:, 1:2], AF.Sqrt, [sta], [sta])
                kb.op("dve", lambda e: e.reciprocal(out=sta[:, 3:4], in_=sta[:, 2:3]), [sta], [sta])
                kb.stt(xt[:, :], xt[:, :], sta[:, 3:4], g[:, :], ALU.mult, ALU.mult, [xt, sta, g], [xt])
                kb.dma("sp", self.out.ap()[t * 128:(t + 1) * 128, :], xt[:, :], reads=[xt], writes=[self.out])
            kb.barrier()

    def dump(self, name, src, rows, cols, dt):
        t = T(self.nc.dram_tensor("dbg_" + name, [rows, cols], dt, kind="ExternalOutput"), name)
        self.kb.dma("sp", t.ap(), src, reads=[], writes=[t])
        self.dbg[name] = t

    def _p(self, fn, *a, **k):
        self.phc += 1
        if self.phc <= self.maxph:
            print("phase", self.phc, fn.__name__, flush=True)
            fn(*a, **k)

    def build(self, nlayers=DEPTH, stop_after=None, maxph=10 ** 9):
        kb, TL, NT = self.kb, self.TL, self.NT
        self.phc = 0
        self.maxph = maxph
        self.declare()
        with contextlib.ExitStack() as st0:
            self.load_consts(st0)
            self.prep_layer_weights(0)
            self._p(self.phase_ada)
            xcur, ycur = self.x_own, self.ctx_in
            for l in range(nlayers):
                need_ctx = l < DEPTH - 1
                if l + 1 < DEPTH:
                    self.prep_layer_weights(l + 1)
                if l % 2 == 0:
                    self._p(self.phase_halo, xcur)
                srcs = [(0, xcur, 0, 0, NT), (1, ycur, 0, TL + 256, 2)]
                if l % 2 == 0:
                    srcs.append((0, self.xhalo, 0, TL, 2))
                self._p(self.phase_norm, srcs, l, l, 0, 1)
                if l % 2 == 0:
                    self._p(self.phase_even_in, l, need_ctx)
                    self._p(self.phase_even_attn, l, need_ctx)
                    self._p(self.phase_even_conv, l, need_ctx)
                    self._p(self.phase_out_gemm, "ev_w_out", l // 2, 32, self.mixT, l, 2, xcur, self.xres[0], ycur, self.yres[0], need_ctx, 512)
                else:
                    self._p(self.phase_odd_dn, l, need_ctx)
                    self._p(self.phase_odd_qkv, l, need_ctx)
                    self._p(self.phase_mla_attn, l, need_ctx)
                    self._p(self.phase_otok_T, need_ctx)
                    self._p(self.phase_out_gemm, "od_w_o", l // 2, 32, self.mixT, l, 2, xcur, self.xres[0], ycur, self.yres[0], need_ctx, 512)
                xcur = self.xres[0]
                if need_ctx:
                    ycur = self.yres[0]
                if stop_after == ("mix", l):
                    break
                with contextlib.ExitStack() as stm:
                    affT = kb.T_sb(stm, "affT", [NE, self.TS], F32)
                    srcs = [(0, xcur, 0, 0, NT)]
                    if need_ctx:
                        srcs.append((1, ycur, 0, TL + 256, 2))
                    self._p(self.phase_norm, srcs, l, DEPTH + l, 3, 4, router=dict(affT=affT))
                    self._p(self.phase_route, stm, affT, need_ctx)
                self._p(self.phase_moe_up, l, need_ctx)
                self._p(self.phase_out_gemm, "moe_d", l, 48, self.HT, l, 5, xcur, self.xres[1], ycur, self.yres[1], need_ctx, 256)
                xcur = self.xres[1]
                if need_ctx:
                    ycur = self.yres[1]
            if self.debug:
                self.dump("x", xcur.ap(), TL, self.D, F32)
                self.dump("y", ycur.ap(), CT, self.D, F32)
                kb.barrier()
            self._p(self.phase_final, xcur)
        return self.nc


def _consts(D, TL, r):
    SEQ = TL * NCORE
    f = np.float32
    c = {}
    c["ident"] = np.eye(128, dtype=f)
    rotE = np.zeros((128, 128), f)
    for m in range(128):
        if (m % 64) < 32:
            rotE[m + 32, m] = -1.0
        else:
            rotE[m - 32, m] = 1.0
    c["rotE"] = rotE
    rotM = np.zeros((64, 64), f)
    for m in range(64):
        if (m % 32) < 16:
            rotM[m + 16, m] = -1.0
        else:
            rotM[m - 16, m] = 1.0
    c["rotM"] = rotM
    p = np.arange(128)
    c["m16"] = (p[:, None] % 16 == p[None, :] % 16).astype(f)
    sel = np.zeros((16, 16, 128), f)
    for e in range(16):
        sel[e, e, :] = 1.0
    c["sel16"] = sel.reshape(16, 16 * 128)
    c["triA"] = np.tile((p[:, None] >= p[None, :]).astype(f), (1, 4))
    c["triB"] = np.tile((p[:, None] <= p[None, :]).astype(f), (1, 4))
    pos = np.concatenate([r * TL + np.arange(TL), r * TL - 128 + np.arange(128), (r + 1) * TL + np.arange(128)])
    pos = np.clip(pos, 0, SEQ - 1)
    rows = (pos // 64).astype(f)
    cols = (pos % 64).astype(f)
    inv = (f(10000.0) ** (-np.arange(0, 64, 2, dtype=f) / f(64))).astype(f)
    ar = (rows[None, :] * inv[:, None]).astype(f)
    ac = (cols[None, :] * inv[:, None]).astype(f)
    c["cosE"] = np.concatenate([np.cos(ar), np.cos(ar), np.cos(ac), np.cos(ac)], 0).astype(f)
    c["sinE"] = np.concatenate([np.sin(ar), np.sin(ar), np.sin(ac), np.sin(ac)], 0).astype(f)
    inv2 = (f(10000.0) ** (-np.arange(0, 32, 2, dtype=f) / f(32))).astype(f)
    ar = (rows[None, :TL] * inv2[:, None]).astype(f)
    ac = (cols[None, :TL] * inv2[:, None]).astype(f)
    c["cosM"] = np.concatenate([np.cos(ar), np.cos(ar), np.cos(ac), np.cos(ac)], 0).astype(f)
    c["sinM"] = np.concatenate([np.sin(ar), np.sin(ar), np.sin(ac), np.sin(ac)], 0).astype(f)
    pv, nv = f(r > 0), f(r < NCORE - 1)
    c["tmask"] = np.concatenate([np.full(128, pv, f), np.full(128, nv, f)])[None, :]
    c["kmask"] = np.tile(np.array([[pv, nv]], f), (128, 1))
    ohp = np.zeros((128, 8), f)
    ohn = np.zeros((128, 8), f)
    if r > 0:
        ohp[:, r - 1] = 1.0
    if r < NCORE - 1:
        ohn[:, r + 1] = 1.0
    c["ohp"], c["ohn"] = ohp, ohn
    return c


def make_in_maps(D, TL, inp):
    f = np.float32
    KC = D // 128
    DS = D // NCORE
    a = {k: np.asarray(v) for k, v in inp.items()}
    shared = {}
    shared["ctx"] = np.concatenate([a["ctx"][0].astype(f), np.zeros((1, D), f)], 0)
    cT = np.stack([a["c"][0].reshape(KC, 128).T, a["c_ctx"].reshape(KC, 128).T], -1)
    shared["cT"] = np.ascontiguousarray(cT, f)
    shared["norm_g"] = np.ascontiguousarray(np.concatenate([a["norm1_g"], a["norm2_g"], a["final_g"][None]], 0), f)
    shared["router"] = np.ascontiguousarray(a["moe_router"], f)
    shared["sink"] = np.ascontiguousarray(a["ev_sink"], f)
    shared["dwT"] = np.ascontiguousarray(a["ev_dw"].reshape(2, B_W, 16, 128).transpose(0, 3, 2, 1), f)
    shared["lngT"] = np.ascontiguousarray(a["ev_ln_g"].reshape(2, 16, 128).transpose(0, 2, 1), f)
    shared["lnbT"] = np.ascontiguousarray(a["ev_ln_b"].reshape(2, 16, 128).transpose(0, 2, 1), f)
    shared["qngT"] = np.ascontiguousarray(a["od_q_norm_g"].reshape(2, 8, 128).transpose(0, 2, 1), f)
    shared["kvngT"] = np.ascontiguousarray(a["od_kv_norm_g"].reshape(2, 4, 128).transpose(0, 2, 1), f)
    big = []
    for l in range(2):
        big += [(f"ev_w_in{l}", a["ev_w_in"][l]), (f"ev_w_out{l}", a["ev_w_out"][l]), (f"od_w_dn{l}", a["od_w_dn"][l]),
                (f"od_w_uq{l}", a["od_w_uq"][l]), (f"od_w_ukv{l}", a["od_w_ukv"][l]), (f"od_w_o{l}", a["od_w_o"][l])]
    for l in range(DEPTH):
        big += [(f"moe_g{l}", a["moe_w_gate"][l].reshape(NE * D, EFF)), (f"moe_u{l}", a["moe_w_up"][l].reshape(NE * D, EFF)),
                (f"moe_d{l}", a["moe_w_down"][l].reshape(NE * EFF, D))]
    adaw = a["ada_w"].reshape(DEPTH, D, 6, NCORE, DS)
    adab = a["ada_b"].reshape(DEPTH, 6, NCORE, DS)
    maps = []
    for r in range(NCORE):
        m = dict(shared)
        m["x_own"] = np.concatenate([a["x"][0, r * TL:(r + 1) * TL].astype(f), np.zeros((1, D), f)], 0)
        m["ada_w_s"] = np.ascontiguousarray(adaw[:, :, :, r, :].reshape(DEPTH, D, 6 * DS), f)
        m["ada_b_s"] = np.ascontiguousarray(adab[:, :, r, :].reshape(DEPTH, 1, 6 * DS), f)
        for name, wfull in big:
            n = wfull.shape[0] // NCORE
            m[name] = np.ascontiguousarray(wfull[r * n:(r + 1) * n], f)
        m.update(_consts(D, TL, r))
        maps.append(m)
    return maps


_CACHE = {}


def run(D, TL, inp, debug=False, nlayers=DEPTH, stop_after=None, trace=False, maxph=10 ** 9):
    key = (D, TL, debug, nlayers, stop_after, maxph)
    if key not in _CACHE:
        mk = MK(D, TL, debug=debug)
        mk.build(nlayers=nlayers, stop_after=stop_after, maxph=maxph)
        _CACHE[key] = mk
    mk = _CACHE[key]
    maps = make_in_maps(D, TL, inp)
    res = run_bass_kernel_spmd(mk.nc, maps, core_ids=list(range(NCORE)), trace=trace)
    return mk, res


def kernel(**inputs):
    x = np.asarray(inputs["x"])
    D = x.shape[-1]
    TL = x.shape[1] // NCORE
    mk, res = run(D, TL, inputs)
    out = np.concatenate([np.asarray(res.results[r]["out"]) for r in range(NCORE)], 0)
    return out.reshape(1, TL * NCORE, D).astype(np.float32)
```
